# Optimizing a Trainium2 kernel written in Bass

```python
import math
import jax
import jax.numpy as jnp
from jax import lax
import numpy as np

D_MODEL = 2048
BATCH = 8
SEQ = 2048
DEPTH = 1
DEC_BATCH = 8
DEC_SEQ = 16
PAST_LEN = 2048

CHUNK = 64
Q_BLOCK = 128
HEAD_DIM = 128
N_SB_HEADS = D_MODEL // 256
N_DF_HEADS = D_MODEL // 512
SB_WIDTH = N_SB_HEADS * HEAD_DIM
DF_WIDTH = N_DF_HEADS * 2 * HEAD_DIM
IN_SPLITS = (SB_WIDTH, 2 * SB_WIDTH, 3 * SB_WIDTH, 3 * SB_WIDTH + DF_WIDTH,
             3 * SB_WIDTH + 2 * DF_WIDTH, 3 * SB_WIDTH + 3 * DF_WIDTH)
IN_WIDTH = 3 * SB_WIDTH + 3 * DF_WIDTH + 2 * D_MODEL
ROPE_THETA = 10000.0
N_GROUPS = 4
EXPERTS_PER_GROUP = 8
N_EXPERTS = N_GROUPS * EXPERTS_PER_GROUP
TOP_K = 2
D_EXPERT = D_MODEL // 2
EXPERT_BLOCK = 128
EPS = 1e-6
NEG_INF = -1e30

kernel_name = 'stickbreak_diffattn_hmoe_stream_step'


def rmsnorm(x, g):
    xf = x.astype(jnp.float32)
    y = xf * lax.rsqrt(jnp.mean(xf * xf, axis=-1, keepdims=True) + EPS)
    return (y * g.astype(jnp.float32)).astype(x.dtype)


def rope(x, pos):
    half = HEAD_DIM // 2
    inv = jnp.exp(-math.log(ROPE_THETA) * jnp.arange(half, dtype=jnp.float32) / half)
    ang = pos.astype(jnp.float32)[:, None] * inv[None, :]
    shape = (1, pos.shape[0]) + (1,) * (x.ndim - 3) + (half,)
    cos = jnp.cos(ang).reshape(shape)
    sin = jnp.sin(ang).reshape(shape)
    xf = x.astype(jnp.float32)
    x1, x2 = xf[..., :half], xf[..., half:]
    return jnp.concatenate([x1 * cos - x2 * sin, x2 * cos + x1 * sin], axis=-1).astype(x.dtype)


def stick_breaking(q, k, v, q_pos, k_pos):
    z = jnp.einsum('bqhd,bkhd->bhqk', q, k).astype(jnp.float32) * (HEAD_DIM ** -0.5)
    mask = k_pos[None, :] < q_pos[:, None]
    log_beta = jax.nn.log_sigmoid(z)
    log_1mb = jnp.where(mask, jax.nn.log_sigmoid(-z), 0.0)
    suffix = lax.cumsum(log_1mb, axis=3, reverse=True) - log_1mb
    a = jnp.where(mask, jnp.exp(log_beta + suffix), 0.0)
    return jnp.einsum('bhqk,bkhd->bqhd', a, v.astype(jnp.float32)).astype(v.dtype)


def diff_attention(q, k, v, q_pos, k_pos, lam):
    s = jnp.einsum('bqhcd,bkhcd->bhcqk', q, k).astype(jnp.float32) * (HEAD_DIM ** -0.5)
    mask = (k_pos[None, :] // CHUNK) <= (q_pos[:, None] // CHUNK)
    p = jax.nn.softmax(jnp.where(mask, s, NEG_INF), axis=-1)
    attn = p[:, :, 0] - lam * p[:, :, 1]
    return jnp.einsum('bhqk,bkhe->bqhe', attn, v.astype(jnp.float32)).astype(v.dtype)


def sweep_query_blocks(attend, q, q_pos):
    b, s = q.shape[:2]
    nb = s // Q_BLOCK
    qb = jnp.moveaxis(q.reshape((b, nb, Q_BLOCK) + q.shape[2:]), 1, 0)
    pb = q_pos.reshape(nb, Q_BLOCK)
    out = lax.map(lambda a: attend(a[0], a[1]), (qb, pb))
    return jnp.moveaxis(out, 0, 1).reshape((b, s) + out.shape[3:])


def token_mixer(h, pos, past, lam_init, w_in, w_pa, w_pb, w_out,
                lam_q1, lam_k1, lam_q2, lam_k2, g_subln):
    b, s, _ = h.shape
    proj = h @ w_in
    q_sb, k_sb, v_sb, q_df, k_df, v_df, gates = jnp.split(proj, IN_SPLITS, axis=-1)
    q_sb = q_sb.reshape(b, s, N_SB_HEADS, HEAD_DIM)
    k_sb = k_sb.reshape(b, s, N_SB_HEADS, HEAD_DIM)
    v_sb = v_sb.reshape(b, s, N_SB_HEADS, HEAD_DIM)
    q_df = rope(q_df.reshape(b, s, N_DF_HEADS, 2, HEAD_DIM), pos)
    k_df = rope(k_df.reshape(b, s, N_DF_HEADS, 2, HEAD_DIM), pos)
    v_df = v_df.reshape(b, s, N_DF_HEADS, 2 * HEAD_DIM)
    lam = (jnp.exp(jnp.sum(lam_q1.astype(jnp.float32) * lam_k1.astype(jnp.float32)))
           - jnp.exp(jnp.sum(lam_q2.astype(jnp.float32) * lam_k2.astype(jnp.float32))) + lam_init)
    if past is None:
        sb_out = sweep_query_blocks(lambda qb, pb: stick_breaking(qb, k_sb, v_sb, pb, pos), q_sb, pos)
        df_out = sweep_query_blocks(lambda qb, pb: diff_attention(qb, k_df, v_df, pb, pos, lam), q_df, pos)
    else:
        cache_sb_k, cache_sb_v, cache_df_k, cache_df_v = past
        k_pos = jnp.concatenate([jnp.arange(cache_sb_k.shape[1], dtype=pos.dtype), pos])
        sb_out = stick_breaking(q_sb, jnp.concatenate([cache_sb_k, k_sb], axis=1),
                                jnp.concatenate([cache_sb_v, v_sb], axis=1), pos, k_pos)
        df_out = diff_attention(q_df, jnp.concatenate([cache_df_k, k_df], axis=1),
                                jnp.concatenate([cache_df_v, v_df], axis=1), pos, k_pos, lam)
    df_out = rmsnorm(df_out, g_subln) * (1.0 - lam_init)
    o_sb = sb_out.reshape(b, s, SB_WIDTH) @ w_pa
    o_df = df_out.reshape(b, s, DF_WIDTH) @ w_pb
    g_sb, g_df = jnp.split(jax.nn.sigmoid(gates), 2, axis=-1)
    y = (g_sb * o_sb + g_df * o_df) @ w_out
    return y, (k_sb, v_sb, k_df, v_df)


def hier_moe(h, w_rg, b_rg, w_re, b_re, w_e_gate, w_e_up, w_e_down):
    b, s, d = h.shape
    t_num = b * s
    t = h.reshape(t_num, d)
    p_g = jax.nn.softmax((t @ w_rg + b_rg).astype(jnp.float32), axis=-1)
    g_idx = jnp.argmax(p_g, axis=-1).astype(jnp.int32)
    p_gsel = jnp.take_along_axis(p_g, g_idx[:, None], axis=-1)
    e_logits = (t @ w_re + b_re).astype(jnp.float32).reshape(t_num, N_GROUPS, EXPERTS_PER_GROUP)
    e_logits = jnp.take_along_axis(e_logits, g_idx[:, None, None], axis=1)[:, 0]
    top_p, top_i = lax.top_k(jax.nn.softmax(e_logits, axis=-1), TOP_K)
    gate_w = p_gsel * top_p / jnp.sum(top_p, axis=-1, keepdims=True)
    e_idx = g_idx[:, None] * EXPERTS_PER_GROUP + top_i.astype(jnp.int32)
    n = t_num * TOP_K
    flat_e = e_idx.reshape(n)
    flat_w = gate_w.reshape(n)
    flat_tok = jnp.repeat(jnp.arange(t_num, dtype=jnp.int32), TOP_K)
    order = jnp.argsort(flat_e).astype(jnp.int32)
    sorted_e = flat_e[order]
    counts = jnp.zeros((N_EXPERTS,), jnp.int32).at[flat_e].add(1)
    padded = (counts + EXPERT_BLOCK - 1) // EXPERT_BLOCK * EXPERT_BLOCK
    ends_pad = jnp.cumsum(padded)
    starts = jnp.cumsum(counts) - counts
    dest = (ends_pad - padded)[sorted_e] + jnp.arange(n, dtype=jnp.int32) - starts[sorted_e]
    n_pad = ((n + N_EXPERTS * (EXPERT_BLOCK - 1) + EXPERT_BLOCK - 1) // EXPERT_BLOCK) * EXPERT_BLOCK
    n_blocks = n_pad // EXPERT_BLOCK
    slot_src = jnp.full((n_pad,), n, jnp.int32).at[dest].set(order)
    slot_tok = jnp.concatenate([flat_tok, jnp.full((1,), t_num, jnp.int32)])[slot_src]
    slot_w = jnp.concatenate([flat_w, jnp.zeros((1,), flat_w.dtype)])[slot_src]
    blk_e = jnp.minimum(jnp.searchsorted(ends_pad, jnp.arange(n_blocks, dtype=jnp.int32) * EXPERT_BLOCK,
                                         side='right'), N_EXPERTS - 1)
    t_pad = jnp.concatenate([t, jnp.zeros((1, d), t.dtype)], axis=0)
    xb = t_pad[slot_tok].reshape(n_blocks, EXPERT_BLOCK, d)

    def expert_block(args):
        xe, e = args
        return (jax.nn.silu(xe @ w_e_gate[e]) * (xe @ w_e_up[e])) @ w_e_down[e]

    yb = lax.map(expert_block, (xb, blk_e)).reshape(n_pad, d)
    out = jax.ops.segment_sum(yb.astype(jnp.float32) * slot_w[:, None], slot_tok,
                              num_segments=t_num + 1)[:t_num]
    return out.reshape(b, s, d).astype(h.dtype)


def trunk_layer(x, c, pos, past, lam_init, w_ada, b_ada, g_mix, w_in, w_pa, w_pb, w_out,
                lam_q1, lam_k1, lam_q2, lam_k2, g_subln, g_moe, w_rg, b_rg, w_re, b_re,
                w_e_gate, w_e_up, w_e_down):
    mod = (c @ w_ada + b_ada)[:, None, :]
    sh1, sc1, gt1, sh2, sc2, gt2 = jnp.split(mod, 6, axis=-1)
    h = rmsnorm(x, g_mix) * (1.0 + sc1) + sh1
    y, rows = token_mixer(h, pos, past, lam_init, w_in, w_pa, w_pb, w_out,
                          lam_q1, lam_k1, lam_q2, lam_k2, g_subln)
    x = x + gt1 * y
    h = rmsnorm(x, g_moe) * (1.0 + sc2) + sh2
    x = x + gt2 * hier_moe(h, w_rg, b_rg, w_re, b_re, w_e_gate, w_e_up, w_e_down)
    return x, rows


def _normal(k, shape, scale):
    return jax.random.normal(k, shape, jnp.float32) * scale


def setup_inputs(seed: int = 0) -> dict:
    key = jax.random.key(seed)
    ks = jax.random.split(key, 32)
    d = D_MODEL
    return {
        'x_prompt': _normal(ks[0], (BATCH, SEQ, d), 1.0),
        'x_sample': _normal(ks[1], (DEC_BATCH, DEC_SEQ, d), 1.0),
        'cache_sb_k': _normal(ks[2], (DEPTH, DEC_BATCH, PAST_LEN, N_SB_HEADS, HEAD_DIM), 1.0),
        'cache_sb_v': _normal(ks[3], (DEPTH, DEC_BATCH, PAST_LEN, N_SB_HEADS, HEAD_DIM), 1.0),
        'cache_df_k': _normal(ks[4], (DEPTH, DEC_BATCH, PAST_LEN, N_DF_HEADS, 2, HEAD_DIM), 1.0),
        'cache_df_v': _normal(ks[5], (DEPTH, DEC_BATCH, PAST_LEN, N_DF_HEADS, 2 * HEAD_DIM), 1.0),
        'c_prompt': _normal(ks[6], (BATCH, d), 1.0),
        'c_sample': _normal(ks[7], (DEC_BATCH, d), 1.0),
        'w_ada': _normal(ks[8], (DEPTH, d, 6 * d), 0.5 * d ** -0.5),
        'b_ada': _normal(ks[9], (DEPTH, 6 * d), 0.01),
        'g_mix': 1.0 + _normal(ks[10], (DEPTH, d), 0.01),
        'w_in': _normal(ks[11], (DEPTH, d, IN_WIDTH), d ** -0.5),
        'w_pa': _normal(ks[12], (DEPTH, SB_WIDTH, d), SB_WIDTH ** -0.5),
        'w_pb': _normal(ks[13], (DEPTH, DF_WIDTH, d), DF_WIDTH ** -0.5),
        'w_out': _normal(ks[14], (DEPTH, d, d), d ** -0.5),
        'lam_q1': _normal(ks[15], (DEPTH, HEAD_DIM), 0.1),
        'lam_k1': _normal(ks[16], (DEPTH, HEAD_DIM), 0.1),
        'lam_q2': _normal(ks[17], (DEPTH, HEAD_DIM), 0.1),
        'lam_k2': _normal(ks[18], (DEPTH, HEAD_DIM), 0.1),
        'g_subln': 1.0 + _normal(ks[19], (DEPTH, 2 * HEAD_DIM), 0.01),
        'g_moe': 1.0 + _normal(ks[20], (DEPTH, d), 0.01),
        'w_rg': _normal(ks[21], (DEPTH, d, N_GROUPS), d ** -0.5),
        'b_rg': _normal(ks[22], (DEPTH, N_GROUPS), 0.01),
        'w_re': _normal(ks[23], (DEPTH, d, N_EXPERTS), d ** -0.5),
        'b_re': _normal(ks[24], (DEPTH, N_EXPERTS), 0.01),
        'w_e_gate': _normal(ks[25], (DEPTH, N_EXPERTS, d, D_EXPERT), d ** -0.5),
        'w_e_up': _normal(ks[26], (DEPTH, N_EXPERTS, d, D_EXPERT), d ** -0.5),
        'w_e_down': _normal(ks[27], (DEPTH, N_EXPERTS, D_EXPERT, d), D_EXPERT ** -0.5),
        'g_final': 1.0 + _normal(ks[28], (d,), 0.01),
    }


def reference(x_prompt, x_sample, cache_sb_k, cache_sb_v, cache_df_k, cache_df_v, c_prompt, c_sample,
              w_ada, b_ada, g_mix, w_in, w_pa, w_pb, w_out, lam_q1, lam_k1, lam_q2, lam_k2, g_subln,
              g_moe, w_rg, b_rg, w_re, b_re, w_e_gate, w_e_up, w_e_down, g_final):
    pos_p = jnp.arange(x_prompt.shape[1], dtype=jnp.int32)
    pos_s = cache_sb_k.shape[2] + jnp.arange(x_sample.shape[1], dtype=jnp.int32)
    xp, xs = x_prompt, x_sample
    sbk_p, sbv_p, dfk_p, dfv_p = [], [], [], []
    sbk_s, sbv_s, dfk_s, dfv_s = [], [], [], []
    for l in range(DEPTH):
        lam_init = 0.8 - 0.6 * math.exp(-0.3 * l)
        lw = (w_ada[l], b_ada[l], g_mix[l], w_in[l], w_pa[l], w_pb[l], w_out[l],
              lam_q1[l], lam_k1[l], lam_q2[l], lam_k2[l], g_subln[l], g_moe[l],
              w_rg[l], b_rg[l], w_re[l], b_re[l], w_e_gate[l], w_e_up[l], w_e_down[l])
        xp, rp = trunk_layer(xp, c_prompt, pos_p, None, lam_init, *lw)
        xs, rs = trunk_layer(xs, c_sample, pos_s,
                             (cache_sb_k[l], cache_sb_v[l], cache_df_k[l], cache_df_v[l]), lam_init, *lw)
        sbk_p.append(rp[0]); sbv_p.append(rp[1]); dfk_p.append(rp[2]); dfv_p.append(rp[3])
        sbk_s.append(rs[0]); sbv_s.append(rs[1]); dfk_s.append(rs[2]); dfv_s.append(rs[3])
    y_prompt = rmsnorm(xp, g_final)
    y_sample = rmsnorm(xs, g_final)
    new_sb_k_prompt = jnp.stack(sbk_p)
    new_sb_v_prompt = jnp.stack(sbv_p)
    new_df_k_prompt = jnp.stack(dfk_p)
    new_df_v_prompt = jnp.stack(dfv_p)
    new_sb_k_sample = jnp.stack(sbk_s)
    new_sb_v_sample = jnp.stack(sbv_s)
    new_df_k_sample = jnp.stack(dfk_s)
    new_df_v_sample = jnp.stack(dfv_s)
    return (y_prompt, y_sample, new_sb_k_prompt, new_sb_v_prompt, new_df_k_prompt, new_df_v_prompt,
            new_sb_k_sample, new_sb_v_sample, new_df_k_sample, new_df_v_sample)
```

```python
import math
from contextlib import ExitStack
import numpy as np
import concourse.bass as bass
import concourse.mybir as mybir
from concourse.bass_utils import run_bass_kernel_spmd

F32 = mybir.dt.float32
BF16 = mybir.dt.bfloat16
I32 = mybir.dt.int32
AF = mybir.ActivationFunctionType
ALU = mybir.AluOpType
AX = mybir.AxisListType

D = 2048
S = 2048
SD = 16
NT = 17
TOK = NT * 128
KC = 16
INW = 10240
EPS = 1e-6
NBLK = 64
NSLOT = NBLK * 128
SCALE = 128 ** -0.5
LAM_INIT = 0.8 - 0.6 * math.exp(0.0)

STAGE = 99


class Buf:
    def __init__(self, name, excl=False):
        self.name = name
        self.excl = excl
        self.writers = []
        self.readers = []
        self.dsem = None
        self.dcnt = 0


class Eng:
    def __init__(self, key):
        self.key = key
        self.ops = []
        self.sem = None
        self.cnt = 0
        self.waited = {}


class Prog:
    def __init__(self, nc):
        self.nc = nc
        self.phase = 0
        self.es = None
        self.glob = ExitStack()

    def begin(self):
        self.es = ExitStack()
        self.eng = {k: Eng(k) for k in ("pe", "act", "dve", "pool", "sp")}
        for k, e in self.eng.items():
            e.sem = self.es.enter_context(self.nc.semaphore(f"ph{self.phase}_{k}"))
        self.dma_final = {}
        self.dmap = {}
        self.nxt = {"sw": 0, "hw": 0}

    def _waits(self, eng, reads, writes):
        toks = []
        for b in reads:
            toks += b.writers
            if b.excl:
                toks += b.readers
        for b in writes:
            toks += b.writers + b.readers
        out = {}
        for (s, v) in toks:
            if s is eng.sem and eng.key in ("pe", "sp"):
                continue
            if eng.waited.get(id(s), 0) >= v:
                continue
            if id(s) not in out or out[id(s)][1] < v:
                out[id(s)] = (s, v)
        for (s, v) in out.values():
            eng.waited[id(s)] = v
        return list(out.values())

    def _update(self, tok, reads, writes):
        for b in reads:
            b.readers.append(tok)
        for b in writes:
            b.writers = [tok]
            b.readers = []

    def op(self, ek, fns, reads=(), writes=()):
        eng = self.eng[ek]
        if not isinstance(fns, (list, tuple)):
            fns = [fns]
        waits = self._waits(eng, reads, writes)
        eng.cnt += 1
        tok = (eng.sem, eng.cnt)
        eng.ops.append((waits, list(fns), (eng.sem, 1)))
        self._update(tok, reads, writes)

    def dma(self, qk, fn, sembuf, reads=(), writes=()):
        eng = self.eng[qk]
        kind = "sw" if qk == "pool" else "hw"
        if not hasattr(self, "pools"):
            self.pools = {"sw": [self.glob.enter_context(self.nc.semaphore(f"swd{i}")) for i in range(14)],
                          "hw": [self.glob.enter_context(self.nc.semaphore(f"hwd{i}")) for i in range(20)]}
            self.semval = {}
        key = (id(sembuf), kind)
        if key not in self.dmap:
            idx = self.nxt[kind]; self.nxt[kind] += 1
            self.dmap[key] = self.pools[kind][idx]
        sem = self.dmap[key]
        waits = self._waits(eng, reads, writes)
        self.semval[id(sem)] = self.semval.get(id(sem), 0) + 16
        tok = (sem, self.semval[id(sem)])
        eng.ops.append((waits, [fn], (sem, 16)))
        self._update(tok, reads, writes)
        self.dma_final[id(sem)] = tok

    def end(self):
        nc = self.nc
        finals = list(self.dma_final.values())
        engs = self.eng

        def emit(e, eng, extra=()):
            for waits, fns, inc in eng.ops:
                for (s, v) in waits:
                    e.wait_ge(s, v)
                ins = None
                for f in fns:
                    ins = f(e)
                ins.then_inc(inc[0], inc[1])
            for (s, v) in extra:
                e.wait_ge(s, v)

        with nc.Block() as blk:
            @blk.sync
            def _(e):
                emit(e, engs["sp"], finals)

            @blk.tensor
            def _(e):
                emit(e, engs["pe"])

            @blk.scalar
            def _(e):
                emit(e, engs["act"])

            @blk.vector
            def _(e):
                emit(e, engs["dve"])

            @blk.gpsimd
            def _(e):
                emit(e, engs["pool"])
        self.es.close()
        self.phase += 1


def build_program(stage=None):
    stage = STAGE if stage is None else stage
    nc = bass.Bass("TRN2", target_bir_lowering=False)
    es = ExitStack()

    def din(name, shape, dt=F32):
        return nc.dram_tensor(name, list(shape), dt, kind="ExternalInput").ap()

    def dout(name, shape, dt=F32):
        return nc.dram_tensor(name, list(shape), dt, kind="ExternalOutput").ap()

    def dscr(name, shape, dt=F32):
        return nc.dram_tensor(name, list(shape), dt, kind="Internal").ap()

    x_p = din("x_p", [S, D]); x_s = din("x_s", [SD, D])
    c_sb_k = din("c_sb_k", [S, 1024]); c_sb_v = din("c_sb_v", [S, 1024])
    c_df_k = din("c_df_k", [S, 1024]); c_df_v = din("c_df_v", [S, 1024])
    c_in = din("c_in", [2, D])
    w_ada = din("w_ada", [D, 6 * D]); b_ada = din("b_ada", [1, 6 * D])
    g_mix = din("g_mix", [1, D]); w_in = din("w_in", [D, INW])
    w_pa = din("w_pa", [1024, D]); w_pb = din("w_pb", [1024, D]); w_out = din("w_out", [D, D])
    lamv = din("lamv", [4, 128]); g_subln = din("g_subln", [256, 1])
    g_moe = din("g_moe", [1, D]); w_r = din("w_r", [D, 36]); b_r = din("b_r", [1, 36])
    w_eg = din("w_eg", [32 * 2048, 1024]); w_eu = din("w_eu", [32 * 2048, 1024]); w_ed = din("w_ed", [32 * 1024, 2048])
    g_fin = din("g_fin", [1, D])
    rope_cs = din("rope_cs", [TOK, 128])

    y_p = dout("y_p", [S, D]); y_s = dout("y_s", [SD, D])
    o_sbk_p = dout("o_sbk_p", [S, 1024]); o_sbv_p = dout("o_sbv_p", [S, 1024])
    o_dfk_p = dout("o_dfk_p", [S, 1024]); o_dfv_p = dout("o_dfv_p", [S, 1024])
    o_sbk_s = dout("o_sbk_s", [SD, 1024]); o_sbv_s = dout("o_sbv_s", [SD, 1024])
    o_dfk_s = dout("o_dfk_s", [SD, 1024]); o_dfv_s = dout("o_dfv_s", [SD, 1024])

    modrows = dscr("modrows", [2, 6, D])
    qkT_d = dscr("qkT_d", [4, 8, 128, TOK], BF16)
    v_d = dscr("v_d", [2, TOK, 1024], BF16)
    gates_d = dscr("gates_d", [TOK, 4096], BF16)
    x2_d = dscr("x2_d", [TOK, D])
    h2_d = dscr("h2_d", [TOK, D], BF16)
    xb_d = dscr("xb_d", [NSLOT + 128, D], BF16)
    yb_d = dscr("yb_d", [NSLOT + 128, D])

    P = Prog(nc)

    def sb(name, shape, dt):
        return es.enter_context(nc.sbuf_tensor(name, list(shape), dt))

    ident_b = sb("ident_b", [128, 128], BF16)
    ident_f = sb("ident_f", [128, 128], F32)
    B_ident = Buf("ident")

    def tok_rows(t):
        return 128 if t < 16 else SD

    P.begin()
    with ExitStack() as ph:
        def sbp(name, shape, dt):
            return ph.enter_context(nc.sbuf_tensor(name, list(shape), dt))

        def psp(name, shape, dt):
            return ph.enter_context(nc.psum_tensor(name, list(shape), dt))

        P.op("pool", lambda e: e.memset(ident_b[:], 0.0), writes=[B_ident])
        P.op("pool", lambda e: e.affine_select(out=ident_b[:], in_=ident_b[:], pattern=[[-1, 128]],
                                               compare_op=ALU.not_equal, fill=1.0, base=0, channel_multiplier=1),
             reads=[B_ident], writes=[B_ident])
        P.op("pool", lambda e: e.tensor_copy(out=ident_f[:], in_=ident_b[:]), reads=[B_ident], writes=[B_ident])

        cT32 = sbp("cT32", [128, KC, 2], F32)
        cT = sbp("cT", [128, KC, 2], BF16)
        B_cT32 = Buf("cT32"); B_cT = Buf("cT")
        for g in range(2):
            P.dma("sp", lambda e, g=g: e.dma_start(out=cT32[:, :, g], in_=c_in[g, :].rearrange("(c p) -> p c", p=128),
                                                   allow_slow_non_contiguous=True), B_cT32, writes=[B_cT32])
        P.op("dve", lambda e: e.tensor_copy(out=cT[:], in_=cT32[:]), reads=[B_cT32], writes=[B_cT])

        modsb = sbp("modsb", [2, 6 * D], F32)
        bada = sbp("bada", [2, 6 * D], F32)
        gvec = sbp("gvec", [2, 2, D], F32)
        B_mod = Buf("modsb"); B_bada = Buf("bada"); B_gvec = Buf("gvec")
        for g in range(2):
            P.dma("sp", lambda e, g=g: e.dma_start(out=bada[g:g + 1, :], in_=b_ada), B_bada, writes=[B_bada])
            P.dma("sp", lambda e, g=g: e.dma_start(out=gvec[g:g + 1, 0, :], in_=g_mix), B_gvec, writes=[B_gvec])
            P.dma("sp", lambda e, g=g: e.dma_start(out=gvec[g:g + 1, 1, :], in_=g_moe), B_gvec, writes=[B_gvec])

        NW = 3
        wslots = [sbp(f"w0_{i}", [128, KC, 512], BF16) for i in range(NW)]
        B_w = [Buf(f"w0_{i}") for i in range(NW)]
        pm = [psp(f"pm{i}", [2, 512], F32) for i in range(2)]
        B_pm = [Buf(f"pm{i}") for i in range(2)]
        for j in range(24):
            s = j % NW
            P.dma("pool", lambda e, j=j, s=s: e.dma_start(
                out=wslots[s][:], in_=w_ada[:, 512 * j:512 * (j + 1)].rearrange("(c p) n -> p c n", p=128)),
                B_w[s], writes=[B_w[s]])
            pp = pm[j % 2]; bp = B_pm[j % 2]
            P.op("pe", [lambda e, c=c, s=s, pp=pp: e.matmul(out=pp[:], lhsT=cT[:, c, :], rhs=wslots[s][:, c, :],
                                                              start=(c == 0), stop=(c == KC - 1)) for c in range(KC)],
                 reads=[B_cT, B_w[s]], writes=[bp])
            P.op("dve", lambda e, j=j, pp=pp: e.tensor_tensor(out=modsb[:, 512 * j:512 * (j + 1)], in0=pp[:],
                                                              in1=bada[:, 512 * j:512 * (j + 1)], op=ALU.add),
                 reads=[bp, B_bada], writes=[B_mod])
        rows = bada[:].rearrange("p (k d) -> p k d", k=6)
        B_rows = B_bada

        def msl(k):
            return modsb[:, D * k:D * (k + 1)]
        P.op("dve", lambda e: e.scalar_tensor_tensor(out=rows[:, 0, :], in0=msl(1), scalar=1.0, in1=gvec[:, 0, :],
                                                     op0=ALU.add, op1=ALU.mult), reads=[B_mod, B_gvec], writes=[B_rows])
        P.op("dve", lambda e: e.tensor_copy(out=rows[:, 1, :], in_=msl(0)), reads=[B_mod], writes=[B_rows])
        P.op("dve", lambda e: e.tensor_copy(out=rows[:, 2, :], in_=msl(2)), reads=[B_mod], writes=[B_rows])
        P.op("dve", lambda e: e.scalar_tensor_tensor(out=rows[:, 3, :], in0=msl(4), scalar=1.0, in1=gvec[:, 1, :],
                                                     op0=ALU.add, op1=ALU.mult), reads=[B_mod, B_gvec], writes=[B_rows])
        P.op("dve", lambda e: e.tensor_copy(out=rows[:, 4, :], in_=msl(3)), reads=[B_mod], writes=[B_rows])
        P.op("dve", lambda e: e.tensor_copy(out=rows[:, 5, :], in_=msl(5)), reads=[B_mod], writes=[B_rows])
        P.dma("sp", lambda e: e.dma_start(out=modrows, in_=rows), B_rows, reads=[B_rows])
        P.end()
    if stage <= 0:
        return nc

    def bc_row(src_row):
        return src_row.broadcast(0, 128) if hasattr(src_row, "broadcast") else src_row

    with ExitStack() as outer:
        hT = outer.enter_context(nc.sbuf_tensor("hT", [128, KC, TOK], BF16))
        B_hT = [Buf(f"hT{t}") for t in range(NT)]

        P.begin()
        with ExitStack() as ph:
            def sbp(name, shape, dt):
                return ph.enter_context(nc.sbuf_tensor(name, list(shape), dt))

            def psp(name, shape, dt):
                return ph.enter_context(nc.psum_tensor(name, list(shape), dt))
            bc = sbp("bc1", [128, 2, 2, D], F32)
            B_bc = Buf("bc1")
            for g in range(2):
                for k in range(2):
                    P.dma("sp", lambda e, g=g, k=k: e.dma_start(
                        out=bc[:, g, k, :], in_=modrows[g, k, :].partition_broadcast(128)), B_bc, writes=[B_bc])
            xt = [sbp(f"xt{i}", [128, D], F32) for i in range(2)]
            B_xt = [Buf(f"xt{i}") for i in range(2)]
            junk = sbp("junk", [128, D], F32); B_junk = Buf("junk")
            st = [sbp(f"st{i}", [128, 4], F32) for i in range(2)]
            B_st = [Buf(f"st{i}") for i in range(2)]
            tmp = [sbp(f"tmp{i}", [128, D], F32) for i in range(2)]
            B_tmp = [Buf(f"tmp{i}") for i in range(2)]
            hb = [sbp(f"hb{i}", [128, D], BF16) for i in range(2)]
            B_hb = [Buf(f"hb{i}") for i in range(2)]
            pt = [psp(f"pt{i}", [128, 4, 128], BF16) for i in range(2)]
            B_pt = [Buf(f"pt{i}") for i in range(2)]
            P.op("pool", lambda e: e.memset(xt[0][:], 0.0), writes=[B_xt[0]])
            npt = 0
            for t in [16] + list(range(16)):
                i = 0 if t == 16 else 1
                g = 0 if t < 16 else 1
                if t < 16:
                    P.dma("sp", lambda e, t=t, i=i: e.dma_start(out=xt[i][:], in_=x_p[128 * t:128 * (t + 1), :]),
                          B_xt[i], writes=[B_xt[i]])
                else:
                    P.dma("sp", lambda e, i=i: e.dma_start(out=xt[i][0:SD, :], in_=x_s), B_xt[i], writes=[B_xt[i]])
                P.op("act", lambda e, i=i: e.activation(out=junk[:], in_=xt[i][:], func=AF.Square,
                                                        accum_out=st[i][:, 0:1]),
                     reads=[B_xt[i]], writes=[B_junk, B_st[i]])
                P.op("dve", lambda e, i=i: e.tensor_scalar(out=st[i][:, 1:2], in0=st[i][:, 0:1], scalar1=1.0 / D,
                                                           scalar2=EPS, op0=ALU.mult, op1=ALU.add),
                     reads=[B_st[i]], writes=[B_st[i]])
                P.op("act", lambda e, i=i: e.activation(out=st[i][:, 2:3], in_=st[i][:, 1:2], func=AF.Sqrt),
                     reads=[B_st[i]], writes=[B_st[i]])
                P.op("dve", lambda e, i=i: e.reciprocal(out=st[i][:, 3:4], in_=st[i][:, 2:3]),
                     reads=[B_st[i]], writes=[B_st[i]])
                P.op("dve", lambda e, i=i, g=g: e.scalar_tensor_tensor(
                    out=tmp[i][:], in0=xt[i][:], scalar=st[i][:, 3:4], in1=bc[:, g, 0, :], op0=ALU.mult, op1=ALU.mult),
                    reads=[B_xt[i], B_st[i], B_bc], writes=[B_tmp[i]])
                P.op("pool", lambda e, i=i, g=g: e.tensor_tensor(out=hb[i][:], in0=tmp[i][:], in1=bc[:, g, 1, :],
                                                                 op=ALU.add),
                     reads=[B_tmp[i], B_bc], writes=[B_hb[i]])
                for q4 in range(4):
                    pi = npt % 2; npt += 1
                    P.op("pe", [lambda e, i=i, c=c, pi=pi: e.transpose(out=pt[pi][:, c % 4, :],
                                                                       in_=hb[i][:, 128 * c:128 * (c + 1)],
                                                                       identity=ident_b[:])
                                for c in range(4 * q4, 4 * q4 + 4)],
                         reads=[B_hb[i], B_ident], writes=[B_pt[pi]])
                    P.op("act", lambda e, t=t, q4=q4, pi=pi: e.copy(
                        out=hT[:, 4 * q4:4 * q4 + 4, 128 * t:128 * (t + 1)], in_=pt[pi][:]),
                        reads=[B_pt[pi]], writes=[B_hT[t]])
            P.end()
        if stage <= 1:
            return nc

        P.begin()
        with ExitStack() as ph:
            def sbp(name, shape, dt):
                return ph.enter_context(nc.sbuf_tensor(name, list(shape), dt))

            def psp(name, shape, dt):
                return ph.enter_context(nc.psum_tensor(name, list(shape), dt))
            NW = 3
            wslots = [sbp(f"w2_{i}", [128, KC, 512], BF16) for i in range(NW)]
            B_w = [Buf(f"w2_{i}") for i in range(NW)]
            pj = [psp(f"pj{i}", [128, 512], F32) for i in range(3)]
            B_pj = [Buf(f"pj{i}") for i in range(3)]
            pt = [psp(f"ptq{i}", [128, 4, 128], BF16) for i in range(2)]
            B_pt = [Buf(f"ptq{i}") for i in range(2)]
            cs = sbp("cs", [128, NT, 128], F32); B_cs = Buf("cs")
            P.dma("sp", lambda e: e.dma_start(out=cs[:], in_=rope_cs.rearrange("(t p) n -> p t n", p=128)),
                  B_cs, writes=[B_cs])
            NS = 3
            f32s = [sbp(f"f32s{i}", [128, 512], F32) for i in range(NS)]
            B_f32s = [Buf(f"f32s{i}") for i in range(NS)]
            ra = [sbp(f"ra{i}", [128, 512], F32) for i in range(2)]
            B_ra = [Buf(f"ra{i}") for i in range(2)]
            b16s = [sbp(f"b16s{i}", [128, 512], BF16) for i in range(NS)]
            B_b16s = [Buf(f"b16s{i}") for i in range(NS)]
            tT = [sbp(f"tT{i}", [128, 4, 128], BF16) for i in range(NS)]
            B_tT = [Buf(f"tT{i}") for i in range(NS)]
            it = 0
            pendT = []
            outs_p = {2: o_sbk_p, 3: o_sbk_p, 4: o_sbv_p, 5: o_sbv_p, 8: o_dfk_p, 9: o_dfk_p, 10: o_dfv_p, 11: o_dfv_p}
            outs_s = {2: o_sbk_s, 3: o_sbk_s, 4: o_sbv_s, 5: o_sbv_s, 8: o_dfk_s, 9: o_dfk_s, 10: o_dfv_s, 11: o_dfv_s}
            import os
            _js = [int(v) for v in os.environ.get('PH2_JS', ','.join(str(v) for v in range(20))).split(',')]
            for j in _js:
                s = j % NW
                P.dma("pool", lambda e, j=j, s=s: e.dma_start(
                    out=wslots[s][:], in_=w_in[:, 512 * j:512 * (j + 1)].rearrange("(c p) n -> p c n", p=128)),
                    B_w[s], writes=[B_w[s]])
                for t in range(NT):
                    k = it % 3; k2 = it % 2; ks = it % NS; it += 1
                    nr = tok_rows(t)
                    pp = pj[k]; bp = B_pj[k]
                    P.op("pe", [lambda e, c=c, s=s, pp=pp, t=t: e.matmul(
                        out=pp[:], lhsT=hT[:, c, 128 * t:128 * (t + 1)], rhs=wslots[s][:, c, :],
                        start=(c == 0), stop=(c == KC - 1)) for c in range(KC)],
                        reads=[B_hT[t], B_w[s]], writes=[bp])
                    if pendT:
                        pendT.pop(0)()
                    half = j % 2
                    grp = j // 2
                    if grp >= 6:
                        gc = (j - 12) * 512
                        P.op("act", lambda e, pp=pp, ks=ks: e.activation(out=b16s[ks][:], in_=pp[:], func=AF.Sigmoid),
                             reads=[bp], writes=[B_b16s[ks]])
                        P.dma("sp", lambda e, ks=ks, t=t, gc=gc: e.dma_start(
                            out=gates_d[128 * t:128 * (t + 1), gc:gc + 512], in_=b16s[ks][:]),
                            B_b16s[ks], reads=[B_b16s[ks]])
                        continue
                    rope = grp in (3, 4)
                    need_T = grp in (0, 1, 3, 4)
                    need_out = grp in (1, 2, 4, 5)
                    src32 = None
                    if rope:
                        xa = pp[:].rearrange("p (h two d) -> p h two d", h=4, two=2)
                        cosb = cs[:, t, 0:64]
                        sinb = cs[:, t, 64:128]
                        A = ra[k2]; Bf = f32s[ks]
                        Av = A[:].rearrange("p (h two d) -> p h two d", h=4, two=2)
                        Bv = Bf[:].rearrange("p (h two d) -> p h two d", h=4, two=2)

                        cos4 = cosb.unsqueeze(1).unsqueeze(1).to_broadcast([128, 4, 2, 64])
                        sin4 = sinb.unsqueeze(1).to_broadcast([128, 4, 64])
                        P.op("dve", lambda e, xa=xa, Av=Av, cos4=cos4: e.tensor_tensor(
                            out=Av, in0=xa, in1=cos4, op=ALU.mult), reads=[bp, B_cs], writes=[B_ra[k2]])
                        P.op("dve", [lambda e, xa=xa, Bv=Bv, sin4=sin4: e.tensor_tensor(
                            out=Bv[:, :, 0, :], in0=xa[:, :, 1, :], in1=sin4, op=ALU.mult),
                            lambda e, xa=xa, Bv=Bv, sin4=sin4: e.tensor_tensor(
                            out=Bv[:, :, 1, :], in0=xa[:, :, 0, :], in1=sin4, op=ALU.mult)],
                            reads=[bp, B_cs], writes=[B_f32s[ks]])
                        P.op("pool", [lambda e, Av=Av, Bv=Bv: e.tensor_tensor(out=Bv[:, :, 0, :], in0=Av[:, :, 0, :],
                                                                              in1=Bv[:, :, 0, :], op=ALU.subtract),
                                      lambda e, Av=Av, Bv=Bv: e.tensor_tensor(out=Bv[:, :, 1, :], in0=Av[:, :, 1, :],
                                                                              in1=Bv[:, :, 1, :], op=ALU.add)],
                             reads=[B_ra[k2], B_f32s[ks]], writes=[B_f32s[ks]])
                        src32 = f32s[ks]
                        P.op("act", lambda e, ks=ks: e.copy(out=b16s[ks][:], in_=f32s[ks][:]),
                             reads=[B_f32s[ks]], writes=[B_b16s[ks]])
                    else:
                        if need_out:
                            P.op("act", lambda e, pp=pp, ks=ks: e.copy(out=f32s[ks][:], in_=pp[:]),
                                 reads=[bp], writes=[B_f32s[ks]])
                            src32 = f32s[ks]
                        if need_out and os.environ.get('PH2_V', 'ser') == 'ser':
                            P.op("dve", lambda e, ks=ks: e.tensor_copy(out=b16s[ks][:], in_=f32s[ks][:]),
                                 reads=[B_f32s[ks]], writes=[B_b16s[ks]])
                        else:
                            P.op("dve", lambda e, pp=pp, ks=ks: e.tensor_copy(out=b16s[ks][:], in_=pp[:]),
                                 reads=[bp], writes=[B_b16s[ks]])
                    if need_out and os.environ.get('PH2_V', '') != 'nodma':
                        cc = half * 512
                        if t < 16:
                            dst = outs_p[j][128 * t:128 * (t + 1), cc:cc + 512]
                            P.dma("sp", lambda e, dst=dst, ks=ks: e.dma_start(out=dst, in_=f32s[ks][:]),
                                  B_f32s[ks], reads=[B_f32s[ks]])
                        else:
                            dst = outs_s[j][:, cc:cc + 512]
                            P.dma("sp", lambda e, dst=dst, ks=ks: e.dma_start(out=dst, in_=f32s[ks][0:SD, :]),
                                  B_f32s[ks], reads=[B_f32s[ks]])
                    if need_T:
                        gi = {0: 0, 1: 1, 3: 2, 4: 3}[grp]
                        pi = it % 2

                        def do_T(ks=ks, pi=pi, gi=gi, half=half, t=t):
                            P.op("pe", [lambda e, ks=ks, c=c, pi=pi: e.transpose(
                                out=pt[pi][:, c, :], in_=b16s[ks][:, 128 * c:128 * (c + 1)], identity=ident_b[:])
                                for c in range(4)], reads=[B_b16s[ks], B_ident], writes=[B_pt[pi]])
                            P.op("act", lambda e, ks=ks, pi=pi: e.copy(out=tT[ks][:], in_=pt[pi][:]),
                                 reads=[B_pt[pi]], writes=[B_tT[ks]])
                            P.dma("sp", lambda e, gi=gi, half=half, t=t, ks=ks: e.dma_start(
                                out=qkT_d[gi, 4 * half:4 * half + 4, :, 128 * t:128 * (t + 1)].rearrange("h d n -> d h n"),
                                in_=tT[ks][:]), B_tT[ks], reads=[B_tT[ks]])
                        pendT.append(do_T)
                    else:
                        vi = 0 if grp == 2 else 1
                        P.dma("sp", lambda e, vi=vi, half=half, t=t, ks=ks: e.dma_start(
                            out=v_d[vi, 128 * t:128 * (t + 1), 512 * half:512 * half + 512], in_=b16s[ks][:]),
                            B_b16s[ks], reads=[B_b16s[ks]])
            while pendT:
                pendT.pop(0)()
            P.end()

    if stage <= 2:
        return nc

    kcdfT_d = dscr("kcdfT_d", [8, 128, S], BF16)
    outer2 = ExitStack()
    sboT = outer2.enter_context(nc.sbuf_tensor("sboT", [128, 8, TOK], BF16))
    dfoT = outer2.enter_context(nc.sbuf_tensor("dfoT", [128, 8, TOK], BF16))
    B_sboT = Buf("sboT"); B_dfoT = Buf("dfoT")

    P.begin()
    with ExitStack() as ph:
        def sbp(name, shape, dt):
            return ph.enter_context(nc.sbuf_tensor(name, list(shape), dt))

        def psp(name, shape, dt):
            return ph.enter_context(nc.psum_tensor(name, list(shape), dt))
        P.op("pool", lambda e: e.memset(sboT[:, :, 2048:TOK], 0.0), writes=[B_sboT])
        P.op("pool", lambda e: e.memset(dfoT[:, :, 2048:TOK], 0.0), writes=[B_dfoT])
        Tm = sbp("Tm", [128, 128], BF16); Tc = sbp("Tc", [128, 128], BF16)
        msk = sbp("msk", [128, 4, 512], BF16)
        B_T = Buf("T"); B_msk = Buf("msk")
        P.op("pool", lambda e: e.memset(Tm[:], 1.0), writes=[B_T])
        P.op("pool", lambda e: e.affine_select(out=Tm[:], in_=Tm[:], pattern=[[-1, 128]], compare_op=ALU.is_ge,
                                               fill=0.0, base=0, channel_multiplier=1), reads=[B_T], writes=[B_T])
        P.op("pool", lambda e: e.memset(Tc[:], 1.0), reads=[B_T], writes=[B_T])
        P.op("pool", lambda e: e.affine_select(out=Tc[:], in_=Tc[:], pattern=[[1, 128]], compare_op=ALU.is_ge,
                                               fill=0.0, base=-1, channel_multiplier=-1), reads=[B_T], writes=[B_T])
        P.op("pool", lambda e: e.memset(msk[:], 1.0), writes=[B_msk])
        for d in range(4):
            P.op("pool", lambda e, d=d: e.affine_select(out=msk[:, d, :], in_=msk[:, d, :], pattern=[[1, 512]],
                                                        compare_op=ALU.is_ge, fill=0.0, base=-128 * d - 1,
                                                        channel_multiplier=-1), reads=[B_msk], writes=[B_msk])
        qT = [sbp(f"qT{i}", [128, TOK], BF16) for i in range(2)]
        kT = [sbp(f"kT{i}", [128, TOK], BF16) for i in range(2)]
        vv = [sbp(f"vv{i}", [128, NT, 128], BF16) for i in range(2)]
        kcb = [sbp(f"kcb{i}", [128, 16, 128], BF16) for i in range(2)]
        vcb = [sbp(f"vcb{i}", [128, 16, 128], BF16) for i in range(2)]
        kcT = [sbp(f"kcT{i}", [128, S], BF16) for i in range(2)]
        kdb = [sbp(f"kdb{i}", [128, 16, 128], BF16) for i in range(2)]
        kdT = [sbp(f"kdT{i}", [128, S], BF16) for i in range(2)]
        B_q = [Buf(f"q{i}") for i in range(2)]; B_k = [Buf(f"k{i}") for i in range(2)]
        B_v = [Buf(f"v{i}") for i in range(2)]; B_kcb = [Buf(f"kcb{i}") for i in range(2)]
        B_vcb = [Buf(f"vcb{i}") for i in range(2)]; B_kcT = [Buf(f"kcT{i}") for i in range(2)]
        B_kdb = [Buf(f"kdb{i}") for i in range(2)]; B_kdT = [Buf(f"kdT{i}") for i in range(2)]
        NWK = 5
        e32 = [sbp(f"e32_{i}", [128, 512], F32) for i in range(NWK)]
        Lall = [sbp(f"Lall_{i}", [128, NT, 512], BF16) for i in range(2)]
        B_Lall = [[Buf(f"Lall{i}_{j}") for j in range(NT)] for i in range(2)]
        ones3 = sbp("ones3", [128, 128], BF16)
        P.op("pool", lambda e: e.memset(ones3[:], 1.0), reads=[B_T], writes=[B_T])
        g32 = [sbp(f"g32_{i}", [128, 512], F32) for i in range(NWK)]
        ab = [sbp(f"ab_{i}", [128, 512], BF16) for i in range(NWK)]
        B_e = [Buf(f"e{i}") for i in range(NWK)]
        B_g = [Buf(f"g{i}") for i in range(NWK)]; B_ab = [Buf(f"ab{i}") for i in range(NWK)]
        zps = [psp(f"zps{i}", [128, 512], F32) for i in range(2)]
        B_z = [Buf(f"z{i}") for i in range(2)]
        Cps = [psp(f"Cps{i}", [128, 512], F32) for i in range(2)]; B_C = [Buf(f"C{i}", True) for i in range(2)]
        Ops = [psp(f"Ops{i}", [128, 512], F32) for i in range(2)]; B_O = [Buf(f"O{i}", True) for i in range(2)]
        ptk = [psp(f"ptk{i}", [128, 4, 128], BF16) for i in range(2)]
        B_ptk = [Buf(f"ptk{i}") for i in range(2)]
        cnt = {"w": 0, "z": 0, "p": 0, "s": 0, "c": 0, "o": 0}

        def cache_T(src_b, B_src, dstT, B_dst):
            for q4 in range(4):
                pi = cnt["p"] % 2; cnt["p"] += 1
                P.op("pe", [lambda e, c=c, pi=pi: e.transpose(out=ptk[pi][:, c % 4, :], in_=src_b[:, c, :],
                                                             identity=ident_b[:]) for c in range(4 * q4, 4 * q4 + 4)],
                     reads=[B_src, B_ident], writes=[B_ptk[pi]])
                P.op("dve", lambda e, q4=q4, pi=pi: e.tensor_copy(
                    out=dstT[:, 512 * q4:512 * (q4 + 1)].rearrange("p (c n) -> p c n", c=4), in_=ptk[pi][:]),
                    reads=[B_ptk[pi]], writes=[B_dst])

        def sb_sweep(h, hb, q0, N, keytiles):
            nk = len(keytiles)
            sw = cnt["s"] % 2; cnt["s"] += 1
            oi = cnt["o"] % 2; cnt["o"] += 1

            def stage_a(idx):
                kt_ap, v_ap, m_ap, rk, rv = keytiles[idx]
                w = cnt["w"] % NWK; cnt["w"] += 1
                zi = cnt["z"] % 2; cnt["z"] += 1
                P.op("pe", lambda e, zi=zi, kt_ap=kt_ap: e.matmul(out=zps[zi][:, 0:N], lhsT=kt_ap,
                                                                  rhs=qT[hb][:, q0:q0 + N], start=True, stop=True),
                     reads=[B_q[hb]] + rk, writes=[B_z[zi]])
                P.op("act", lambda e, zi=zi, w=w: e.activation(out=e32[w][:, 0:N], in_=zps[zi][:, 0:N], func=AF.Exp,
                                                               scale=SCALE), reads=[B_z[zi]], writes=[B_e[w]])
                if m_ap is not None:
                    P.op("pool", lambda e, w=w, m_ap=m_ap: e.tensor_tensor(out=e32[w][:, 0:N], in0=e32[w][:, 0:N],
                                                                           in1=m_ap, op=ALU.mult),
                         reads=[B_e[w], B_msk], writes=[B_e[w]])
                P.op("act", lambda e, w=w, idx=idx: e.activation(out=Lall[sw][:, idx, 0:N], in_=e32[w][:, 0:N],
                                                                 func=AF.Ln, bias=1.0),
                     reads=[B_e[w]], writes=[B_Lall[sw][idx]])
                return w

            def stage_b(idx, w):
                ci = cnt["c"] % 2; cnt["c"] += 1
                fl = [lambda e, b=b, ci=ci: e.matmul(out=Cps[ci][:, 0:N], lhsT=ones3[:], rhs=Lall[sw][:, b, 0:N],
                                                     start=(b == 0), stop=False) for b in range(idx)]
                fl.append(lambda e, idx=idx, ci=ci: e.matmul(out=Cps[ci][:, 0:N], lhsT=Tm[:], rhs=Lall[sw][:, idx, 0:N],
                                                             start=(idx == 0), stop=True))
                P.op("pe", fl, reads=[B_Lall[sw][b] for b in range(idx + 1)] + [B_T], writes=[B_C[ci]])
                P.op("act", lambda e, w=w, ci=ci: e.activation(out=g32[w][:, 0:N], in_=Cps[ci][:, 0:N], func=AF.Exp, scale=-1.0),
                     reads=[B_C[ci]], writes=[B_g[w]])
                P.op("dve", lambda e, w=w: e.tensor_tensor(out=ab[w][:, 0:N], in0=e32[w][:, 0:N], in1=g32[w][:, 0:N],
                                                           op=ALU.mult), reads=[B_e[w], B_g[w]], writes=[B_ab[w]])

            def stage_c(idx, w):
                kt_ap, v_ap, m_ap, rk, rv = keytiles[idx]
                first = idx == 0; last = idx == nk - 1
                P.op("pe", lambda e, w=w, v_ap=v_ap, first=first, last=last: e.matmul(
                    out=Ops[oi][:, 0:N], lhsT=v_ap, rhs=ab[w][:, 0:N], start=first, stop=last),
                    reads=[B_ab[w]] + rv, writes=[B_O[oi]])

            ws_ = {}
            for it_ in range(nk + 2):
                if it_ < nk:
                    ws_[it_] = stage_a(it_)
                if 0 <= it_ - 1 < nk:
                    stage_b(it_ - 1, ws_[it_ - 1])
                if 0 <= it_ - 2 < nk:
                    stage_c(it_ - 2, ws_[it_ - 2])
            P.op("dve", lambda e: e.tensor_copy(out=sboT[:, h, q0:q0 + N], in_=Ops[oi][:, 0:N]),
                 reads=[B_O[oi]], writes=[B_sboT])

        for h in range(8):
            hb = h % 2
            P.dma("sp", lambda e, h=h, hb=hb: e.dma_start(out=qT[hb][:], in_=qkT_d[0, h, :, :]), B_q[hb], writes=[B_q[hb]])
            P.dma("sp", lambda e, h=h, hb=hb: e.dma_start(out=kT[hb][:], in_=qkT_d[1, h, :, :]), B_k[hb], writes=[B_k[hb]])
            P.dma("sp", lambda e, h=h, hb=hb: e.dma_start(
                out=vv[hb][:], in_=v_d[0, :, 128 * h:128 * (h + 1)].rearrange("(t p) d -> p t d", p=128)),
                B_v[hb], writes=[B_v[hb]])
            P.dma("pool", lambda e, h=h, hb=hb: e.dma_start(
                out=kcb[hb][:], in_=c_sb_k[:, 128 * h:128 * (h + 1)].rearrange("(t p) d -> p t d", p=128)),
                B_kcb[hb], writes=[B_kcb[hb]])
            P.dma("pool", lambda e, h=h, hb=hb: e.dma_start(
                out=vcb[hb][:], in_=c_sb_v[:, 128 * h:128 * (h + 1)].rearrange("(t p) d -> p t d", p=128)),
                B_vcb[hb], writes=[B_vcb[hb]])
            P.dma("pool", lambda e, h=h, hb=hb: e.dma_start(
                out=kdb[hb][:], in_=c_df_k[:, 128 * h:128 * (h + 1)].rearrange("(t p) d -> p t d", p=128)),
                B_kdb[hb], writes=[B_kdb[hb]])
            cache_T(kcb[hb], B_kcb[hb], kcT[hb], B_kcT[hb])
            cache_T(kdb[hb], B_kdb[hb], kdT[hb], B_kdT[hb])
            P.dma("sp", lambda e, h=h, hb=hb: e.dma_start(out=kcdfT_d[h, :, :], in_=kdT[hb][:]), B_kdT[hb],
                  reads=[B_kdT[hb]])
            for i in range(4):
                kts = []
                for j in range(4 * i + 3, -1, -1):
                    m_ap = msk[:, j - 4 * i, :] if j >= 4 * i else None
                    kts.append((kT[hb][:, 128 * j:128 * (j + 1)], vv[hb][:, j, :], m_ap, [B_k[hb]], [B_v[hb]]))
                sb_sweep(h, hb, 512 * i, 512, kts)
            kts = [(kT[hb][:, 2048:2176], vv[hb][:, 16, :], msk[:, 0, 0:SD], [B_k[hb]], [B_v[hb]])]
            for j in range(15, -1, -1):
                kts.append((kcT[hb][:, 128 * j:128 * (j + 1)], vcb[hb][:, j, :], None, [B_kcT[hb]], [B_vcb[hb]]))
            sb_sweep(h, hb, 2048, SD, kts)
        P.end()
    if stage <= 3:
        pass
        return nc

    P.begin()
    with ExitStack() as ph:
        def sbp(name, shape, dt):
            return ph.enter_context(nc.sbuf_tensor(name, list(shape), dt))

        def psp(name, shape, dt):
            return ph.enter_context(nc.psum_tensor(name, list(shape), dt))
        ones_b = sbp("ones_b", [128, 128], BF16); ones_f = sbp("ones_f", [128, 128], F32)
        mskd = sbp("mskd", [128, 4, 512], BF16); mskv = sbp("mskv", [128, SD], BF16)
        B_cst = Buf("cst4")
        P.op("pool", lambda e: e.memset(ones_b[:], 1.0), writes=[B_cst])
        P.op("pool", lambda e: e.memset(ones_f[:], 1.0), writes=[B_cst])
        P.op("pool", lambda e: e.memset(mskd[:], 1.0), writes=[B_cst])
        for d in range(4):
            P.op("pool", lambda e, d=d: e.affine_select(
                out=mskd[:, d, :].rearrange("p (a b) -> p a b", a=8), in_=mskd[:, d, :].rearrange("p (a b) -> p a b", a=8),
                pattern=[[64, 8], [0, 64]], compare_op=ALU.is_ge, fill=0.0, base=63 - 128 * d, channel_multiplier=-1),
                reads=[B_cst], writes=[B_cst])
        P.op("pool", lambda e: e.memset(mskv[:], 1.0), reads=[B_cst], writes=[B_cst])
        P.op("pool", lambda e: e.affine_select(out=mskv[:], in_=mskv[:], pattern=[[0, SD]], compare_op=ALU.is_ge,
                                               fill=0.0, base=SD - 1, channel_multiplier=-1), reads=[B_cst], writes=[B_cst])
        lt = sbp("lt", [128, 4, 128], F32); lw = sbp("lw", [128, 2, 128], F32); ls = sbp("ls", [128, 8], F32)
        gsc = sbp("gsc", [128, 2], F32)
        B_lt = Buf("lt"); B_ls = Buf("ls"); B_gsc = Buf("gsc")
        for r in range(4):
            P.dma("sp", lambda e, r=r: e.dma_start(out=lt[:, r, :], in_=lamv[r, :].partition_broadcast(128)), B_lt, writes=[B_lt])
        for half in range(2):
            P.dma("sp", lambda e, half=half: e.dma_start(out=gsc[:, half:half + 1], in_=g_subln[128 * half:128 * (half + 1), :]),
                  B_gsc, writes=[B_gsc])
        P.op("dve", lambda e: e.tensor_scalar(out=gsc[:], in0=gsc[:], scalar1=1.0 - LAM_INIT, scalar2=None, op0=ALU.mult),
             reads=[B_gsc], writes=[B_gsc])
        B_lw = Buf("lw")
        P.op("dve", lambda e: e.tensor_tensor(out=lw[:, 0, :], in0=lt[:, 0, :], in1=lt[:, 1, :], op=ALU.mult), reads=[B_lt], writes=[B_lw])
        P.op("dve", lambda e: e.tensor_tensor(out=lw[:, 1, :], in0=lt[:, 2, :], in1=lt[:, 3, :], op=ALU.mult), reads=[B_lw, B_lt], writes=[B_lw])
        P.op("dve", lambda e: e.reduce_sum(out=ls[:, 0:2], in_=lw[:], axis=AX.X), reads=[B_lw], writes=[B_ls])
        P.op("act", lambda e: e.activation(out=ls[:, 2:4], in_=ls[:, 0:2], func=AF.Exp), reads=[B_ls], writes=[B_ls])
        P.op("dve", lambda e: e.tensor_tensor(out=ls[:, 4:5], in0=ls[:, 3:4], in1=ls[:, 2:3], op=ALU.subtract), reads=[B_ls], writes=[B_ls])
        P.op("dve", lambda e: e.tensor_scalar(out=ls[:, 5:6], in0=ls[:, 4:5], scalar1=-LAM_INIT, scalar2=None, op0=ALU.add),
             reads=[B_ls], writes=[B_ls])
        neglam = ls[:, 5:6]

        qT2 = [[sbp(f"dq{i}{c}", [128, TOK], BF16) for c in range(2)] for i in range(2)]
        kT2 = [[sbp(f"dk{i}{c}", [128, TOK], BF16) for c in range(2)] for i in range(2)]
        kc2 = [[sbp(f"dkc{i}{c}", [128, S], BF16) for c in range(2)] for i in range(2)]
        vv2 = [sbp(f"dv{i}", [128, NT, 256], BF16) for i in range(2)]
        vc2 = [sbp(f"dvc{i}", [128, 16, 256], BF16) for i in range(2)]
        B_q2 = [Buf(f"dq{i}") for i in range(2)]; B_k2 = [Buf(f"dk{i}") for i in range(2)]
        B_kc2 = [Buf(f"dkc{i}") for i in range(2)]; B_v2 = [Buf(f"dv{i}") for i in range(2)]
        B_vc2 = [Buf(f"dvc{i}") for i in range(2)]
        NWK = 4
        pb = [sbp(f"pb{i}", [128, 512], BF16) for i in range(NWK)]
        B_pb = [Buf(f"pb{i}") for i in range(NWK)]
        zps = [psp(f"dz{i}", [128, 512], F32) for i in range(2)]
        B_z = [Buf(f"dz{i}") for i in range(2)]
        pv = [[psp(f"pv{c}{hf}", [128, 512], F32) for hf in range(2)] for c in range(2)]
        den = [psp(f"den{c}", [128, 512], F32) for c in range(2)]
        B_acc = [Buf(f"acc{c}") for c in range(2)]
        rden = [sbp(f"rden{c}", [128, 512], F32) for c in range(2)]
        B_rden = [Buf(f"rden{c}") for c in range(2)]
        oo = [[sbp(f"oo{c}{hf}", [128, 512], F32) for hf in range(2)] for c in range(2)]
        B_oo = [Buf(f"oo{c}") for c in range(2)]
        at = [sbp(f"at{hf}", [128, 512], F32) for hf in range(2)]
        sq = [sbp(f"sq{hf}", [128, 512], F32) for hf in range(2)]
        rs = sbp("rs", [128, 512], F32); rs2 = sbp("rs2", [128, 512], F32)
        B_at = Buf("at"); B_sq = Buf("sq"); B_rs = Buf("rs"); B_rs2 = Buf("rs2")
        cnt = {"w": 0, "z": 0}

        def df_sweep(h, hb, q0, N, keytiles):
            nk = len(keytiles)
            steps = [(idx, c) for idx in range(nk) for c in range(2)]
            pend = []

            def stage_a(idx, c):
                kaps, vap, m_ap, rk, rv = keytiles[idx]
                w = cnt["w"] % NWK; cnt["w"] += 1
                zi = cnt["z"] % 2; cnt["z"] += 1
                P.op("pe", lambda e, zi=zi, c=c, kaps=kaps: e.matmul(
                    out=zps[zi][:, 0:N], lhsT=kaps[c], rhs=qT2[hb][c][:, q0:q0 + N], start=True, stop=True),
                    reads=[B_q2[hb]] + rk, writes=[B_z[zi]])
                P.op("act", lambda e, zi=zi, w=w: e.activation(out=pb[w][:, 0:N], in_=zps[zi][:, 0:N], func=AF.Exp,
                                                               scale=SCALE), reads=[B_z[zi]], writes=[B_pb[w]])
                if m_ap is not None:
                    P.op("pool", lambda e, w=w, m_ap=m_ap: e.tensor_tensor(
                        out=pb[w][:, 0:N], in0=pb[w][:, 0:N], in1=m_ap, op=ALU.mult),
                        reads=[B_pb[w], B_cst], writes=[B_pb[w]])
                return w

            def stage_b(idx, c, w):
                kaps, vap, m_ap, rk, rv = keytiles[idx]
                first = idx == 0; last = idx == nk - 1
                P.op("pe", [lambda e, w=w, c=c, vap=vap, first=first, last=last: e.matmul(
                                out=pv[c][0][:, 0:N], lhsT=vap[:, 0:128], rhs=pb[w][:, 0:N], start=first, stop=last),
                            lambda e, w=w, c=c, vap=vap, first=first, last=last: e.matmul(
                                out=pv[c][1][:, 0:N], lhsT=vap[:, 128:256], rhs=pb[w][:, 0:N], start=first, stop=last),
                            lambda e, w=w, c=c, first=first, last=last: e.matmul(
                                out=den[c][:, 0:N], lhsT=ones_b[:], rhs=pb[w][:, 0:N], start=first, stop=last)],
                     reads=[B_pb[w], B_cst] + rv, writes=[B_acc[c]])

            for (idx, c) in steps:
                w = stage_a(idx, c)
                pend.append((idx, c, w))
                if len(pend) > 1:
                    stage_b(*pend.pop(0))
            while pend:
                stage_b(*pend.pop(0))
            for c in range(2):
                P.op("dve", lambda e, c=c: e.reciprocal(out=rden[c][:, 0:N], in_=den[c][:, 0:N]),
                     reads=[B_acc[c]], writes=[B_rden[c]])
                P.op("dve", [lambda e, c=c, hf=hf: e.tensor_tensor(out=oo[c][hf][:, 0:N], in0=pv[c][hf][:, 0:N],
                                                                  in1=rden[c][:, 0:N], op=ALU.mult) for hf in range(2)],
                     reads=[B_acc[c], B_rden[c]], writes=[B_oo[c]])
            P.op("dve", [lambda e, hf=hf: e.scalar_tensor_tensor(out=at[hf][:, 0:N], in0=oo[1][hf][:, 0:N], scalar=neglam,
                                                                 in1=oo[0][hf][:, 0:N], op0=ALU.mult, op1=ALU.add)
                         for hf in range(2)], reads=[B_oo[0], B_oo[1], B_ls], writes=[B_at])
            P.op("act", lambda e: e.activation(out=sq[0][:, 0:N], in_=at[0][:, 0:N], func=AF.Square), reads=[B_at], writes=[B_sq])
            P.op("act", lambda e: e.activation(out=sq[1][:, 0:N], in_=at[1][:, 0:N], func=AF.Square), reads=[B_at, B_sq], writes=[B_sq])
            zi = cnt["z"] % 2; cnt["z"] += 1
            P.op("pe", [lambda e, zi=zi, hf=hf: e.matmul(out=zps[zi][:, 0:N], lhsT=ones_f[:], rhs=sq[hf][:, 0:N],
                                                        start=(hf == 0), stop=(hf == 1)) for hf in range(2)],
                 reads=[B_sq, B_cst], writes=[B_z[zi]])
            P.op("dve", lambda e, zi=zi: e.tensor_scalar(out=rs2[:, 0:N], in0=zps[zi][:, 0:N], scalar1=1.0 / 256, scalar2=EPS,
                                                         op0=ALU.mult, op1=ALU.add), reads=[B_z[zi]], writes=[B_rs2])
            P.op("act", lambda e: e.activation(out=rs[:, 0:N], in_=rs2[:, 0:N], func=AF.Sqrt), reads=[B_rs2], writes=[B_rs])
            P.op("dve", lambda e: e.reciprocal(out=rs2[:, 0:N], in_=rs[:, 0:N]), reads=[B_rs], writes=[B_rs2])
            for hf in range(2):
                P.op("dve", lambda e, hf=hf: e.scalar_tensor_tensor(
                    out=dfoT[:, 2 * h + hf, q0:q0 + N], in0=at[hf][:, 0:N], scalar=gsc[:, hf:hf + 1], in1=rs2[:, 0:N],
                    op0=ALU.mult, op1=ALU.mult), reads=[B_at, B_gsc, B_rs2], writes=[B_dfoT])

        for h in range(4):
            hb = h % 2
            for c in range(2):
                P.dma("sp", lambda e, h=h, hb=hb, c=c: e.dma_start(out=qT2[hb][c][:], in_=qkT_d[2, 2 * h + c, :, :]),
                      B_q2[hb], writes=[B_q2[hb]])
                P.dma("sp", lambda e, h=h, hb=hb, c=c: e.dma_start(out=kT2[hb][c][:], in_=qkT_d[3, 2 * h + c, :, :]),
                      B_k2[hb], writes=[B_k2[hb]])
                P.dma("sp", lambda e, h=h, hb=hb, c=c: e.dma_start(out=kc2[hb][c][:], in_=kcdfT_d[2 * h + c, :, :]),
                      B_kc2[hb], writes=[B_kc2[hb]])
            P.dma("sp", lambda e, h=h, hb=hb: e.dma_start(
                out=vv2[hb][:], in_=v_d[1, :, 256 * h:256 * (h + 1)].rearrange("(t p) d -> p t d", p=128)),
                B_v2[hb], writes=[B_v2[hb]])
            P.dma("pool", lambda e, h=h, hb=hb: e.dma_start(
                out=vc2[hb][:], in_=c_df_v[:, 256 * h:256 * (h + 1)].rearrange("(t p) d -> p t d", p=128)),
                B_vc2[hb], writes=[B_vc2[hb]])
            for i in range(4):
                kts = []
                for j in range(4 * i + 4):
                    m_ap = mskd[:, j - 4 * i, :] if j >= 4 * i else None
                    kts.append(([kT2[hb][c][:, 128 * j:128 * (j + 1)] for c in range(2)], vv2[hb][:, j, :], m_ap,
                                [B_k2[hb]], [B_v2[hb]]))
                df_sweep(h, hb, 512 * i, 512, kts)
            kts = [([kT2[hb][c][:, 2048:2176] for c in range(2)], vv2[hb][:, 16, :], mskv[:], [B_k2[hb]], [B_v2[hb]])]
            for j in range(16):
                kts.append(([kc2[hb][c][:, 128 * j:128 * (j + 1)] for c in range(2)], vc2[hb][:, j, :], None,
                            [B_kc2[hb]], [B_vc2[hb]]))
            df_sweep(h, hb, 2048, SD, kts)
        P.end()
    if stage <= 4:
        pass
        return nc

    mT_d = dscr("mT_d", [KC, 128, TOK], BF16)
    P.begin()
    with ExitStack() as ph:
        def sbp(name, shape, dt):
            return ph.enter_context(nc.sbuf_tensor(name, list(shape), dt))

        def psp(name, shape, dt):
            return ph.enter_context(nc.psum_tensor(name, list(shape), dt))
        wa = [sbp(f"wa{i}", [128, 8, 512], BF16) for i in range(2)]
        wb = [sbp(f"wb{i}", [128, 8, 512], BF16) for i in range(2)]
        B_wa = [Buf(f"wa{i}") for i in range(2)]; B_wb = [Buf(f"wb{i}") for i in range(2)]
        gsb = [sbp(f"gsb{i}", [128, 512], BF16) for i in range(2)]
        gdf = [sbp(f"gdf{i}", [128, 512], BF16) for i in range(2)]
        B_gsb = [Buf(f"gsb{i}") for i in range(2)]; B_gdf = [Buf(f"gdf{i}") for i in range(2)]
        t1 = [sbp(f"t1_{i}", [128, 512], F32) for i in range(2)]
        t2 = [sbp(f"t2_{i}", [128, 512], F32) for i in range(2)]
        mb = [sbp(f"mb_{i}", [128, 512], BF16) for i in range(2)]
        mts = [sbp(f"mts_{i}", [128, 4, 128], BF16) for i in range(2)]
        B_t1 = [Buf(f"t1{i}") for i in range(2)]; B_t2 = [Buf(f"t2{i}") for i in range(2)]
        B_mb = [Buf(f"mb{i}") for i in range(2)]; B_mts = [Buf(f"mts{i}") for i in range(2)]
        pA = [psp(f"pA{i}", [128, 512], F32) for i in range(2)]
        pB = [psp(f"pB{i}", [128, 512], F32) for i in range(2)]
        B_pA = [Buf(f"pA{i}", True) for i in range(2)]; B_pB = [Buf(f"pB{i}", True) for i in range(2)]
        ptm = [psp(f"ptm{i}", [128, 4, 128], BF16) for i in range(2)]
        B_ptm = [Buf(f"ptm{i}", True) for i in range(2)]
        it = 0
        pend5 = []
        for j in range(4):
            s2 = j % 2
            P.dma("pool", lambda e, j=j, s2=s2: e.dma_start(
                out=wa[s2][:], in_=w_pa[:, 512 * j:512 * (j + 1)].rearrange("(c p) n -> p c n", p=128)),
                B_wa[s2], writes=[B_wa[s2]])
            P.dma("pool", lambda e, j=j, s2=s2: e.dma_start(
                out=wb[s2][:], in_=w_pb[:, 512 * j:512 * (j + 1)].rearrange("(c p) n -> p c n", p=128)),
                B_wb[s2], writes=[B_wb[s2]])
            for t in range(NT):
                k = it % 2; it += 1
                P.dma("sp", lambda e, t=t, j=j, k=k: e.dma_start(
                    out=gsb[k][:], in_=gates_d[128 * t:128 * (t + 1), 512 * j:512 * (j + 1)]), B_gsb[k], writes=[B_gsb[k]])
                P.dma("sp", lambda e, t=t, j=j, k=k: e.dma_start(
                    out=gdf[k][:], in_=gates_d[128 * t:128 * (t + 1), 2048 + 512 * j:2048 + 512 * (j + 1)]),
                    B_gdf[k], writes=[B_gdf[k]])
                P.op("pe", [lambda e, c=c, k=k, t=t, s2=s2: e.matmul(
                    out=pA[k][:], lhsT=sboT[:, c, 128 * t:128 * (t + 1)], rhs=wa[s2][:, c, :], start=(c == 0), stop=(c == 7))
                    for c in range(8)], reads=[B_sboT, B_wa[s2]], writes=[B_pA[k]])
                P.op("pe", [lambda e, c=c, k=k, t=t, s2=s2: e.matmul(
                    out=pB[k][:], lhsT=dfoT[:, c, 128 * t:128 * (t + 1)], rhs=wb[s2][:, c, :], start=(c == 0), stop=(c == 7))
                    for c in range(8)], reads=[B_dfoT, B_wb[s2]], writes=[B_pB[k]])
                if pend5:
                    pend5.pop(0)()
                P.op("dve", lambda e, k=k: e.tensor_tensor(out=t1[k][:], in0=pA[k][:], in1=gsb[k][:], op=ALU.mult),
                     reads=[B_pA[k], B_gsb[k]], writes=[B_t1[k]])
                P.op("dve", lambda e, k=k: e.tensor_tensor(out=t2[k][:], in0=pB[k][:], in1=gdf[k][:], op=ALU.mult),
                     reads=[B_pB[k], B_gdf[k]], writes=[B_t2[k]])
                P.op("pool", lambda e, k=k: e.tensor_tensor(out=mb[k][:], in0=t1[k][:], in1=t2[k][:], op=ALU.add),
                     reads=[B_t1[k], B_t2[k]], writes=[B_mb[k]])
                def do_T5(k=k, j=j, t=t):
                    P.op("pe", [lambda e, c=c, k=k: e.transpose(out=ptm[k][:, c, :], in_=mb[k][:, 128 * c:128 * (c + 1)],
                                                                identity=ident_b[:]) for c in range(4)],
                         reads=[B_mb[k], B_ident], writes=[B_ptm[k]])
                    P.op("act", lambda e, k=k: e.copy(out=mts[k][:], in_=ptm[k][:]), reads=[B_ptm[k]], writes=[B_mts[k]])
                    P.dma("sp", lambda e, j=j, t=t, k=k: e.dma_start(
                        out=mT_d[4 * j:4 * j + 4, :, 128 * t:128 * (t + 1)].rearrange("c d n -> d c n"), in_=mts[k][:]),
                        B_mts[k], reads=[B_mts[k]])
                pend5.append(do_T5)
        while pend5:
            pend5.pop(0)()
        P.end()
    outer2.close()
    if stage <= 5:
        return nc

    Mk_all = sb("Mk_all", [128, NT, 2, 32], F32)
    wgt_all = sb("wgt_all", [128, NT, 2], F32)
    desti = sb("desti", [128, NT, 2], I32)
    widx = sb("widx", [128, NBLK, 2], I32)
    B_Mk = Buf("Mk"); B_wgt = Buf("wgt"); B_desti = Buf("desti"); B_widx = Buf("widx")

    P.begin()
    with ExitStack() as ph:
        def sbp(name, shape, dt):
            return ph.enter_context(nc.sbuf_tensor(name, list(shape), dt))

        def psp(name, shape, dt):
            return ph.enter_context(nc.psum_tensor(name, list(shape), dt))
        mT = sbp("mT", [128, KC, TOK], BF16); B_mT = Buf("mT")
        for q4 in range(4):
            P.dma("sp", lambda e, q4=q4: e.dma_start(out=mT[:, 4 * q4:4 * q4 + 4, :],
                                                     in_=mT_d[4 * q4:4 * q4 + 4, :, :].rearrange("c d n -> d c n")),
                  B_mT, writes=[B_mT])
        gt1 = sbp("gt1", [128, 2, D], F32); B_gt1 = Buf("gt1")
        for g in range(2):
            P.dma("sp", lambda e, g=g: e.dma_start(out=gt1[:, g, :], in_=modrows[g, 2, :].partition_broadcast(128)),
                  B_gt1, writes=[B_gt1])
        wo = [sbp(f"wo{i}", [128, KC, 512], BF16) for i in range(2)]
        B_wo = [Buf(f"wo{i}") for i in range(2)]
        xb_ = [sbp(f"xblk{i}", [128, 512], F32) for i in range(3)]
        B_xb = [Buf(f"xblk{i}") for i in range(3)]
        tq = [sbp(f"tq{i}", [128, 512], F32) for i in range(3)]
        B_tq = [Buf(f"tq{i}") for i in range(3)]
        py = [psp(f"py{i}", [128, 512], F32) for i in range(3)]
        B_py = [Buf(f"py{i}", True) for i in range(3)]
        for i in range(3):
            P.op("pool", lambda e, i=i: e.memset(xb_[i][:], 0.0), writes=[B_xb[i]])
        it = 0
        for j in range(4):
            s2 = j % 2
            P.dma("pool", lambda e, j=j, s2=s2: e.dma_start(
                out=wo[s2][:], in_=w_out[:, 512 * j:512 * (j + 1)].rearrange("(c p) n -> p c n", p=128)),
                B_wo[s2], writes=[B_wo[s2]])
            for t in range(NT):
                k = it % 3; it += 1
                g = 0 if t < 16 else 1
                if t < 16:
                    P.dma("sp", lambda e, t=t, j=j, k=k: e.dma_start(
                        out=xb_[k][:], in_=x_p[128 * t:128 * (t + 1), 512 * j:512 * (j + 1)]), B_xb[k], writes=[B_xb[k]])
                else:
                    P.dma("sp", lambda e, j=j, k=k: e.dma_start(out=xb_[k][0:SD, :], in_=x_s[:, 512 * j:512 * (j + 1)]),
                          B_xb[k], writes=[B_xb[k]])
                P.op("pe", [lambda e, c=c, k=k, t=t, s2=s2: e.matmul(
                    out=py[k][:], lhsT=mT[:, c, 128 * t:128 * (t + 1)], rhs=wo[s2][:, c, :], start=(c == 0), stop=(c == KC - 1))
                    for c in range(KC)], reads=[B_mT, B_wo[s2]], writes=[B_py[k]])
                P.op("dve", lambda e, k=k, g=g, j=j: e.tensor_tensor(out=tq[k][:], in0=py[k][:],
                                                                     in1=gt1[:, g, 512 * j:512 * (j + 1)], op=ALU.mult),
                     reads=[B_py[k], B_gt1], writes=[B_tq[k]])
                P.op("pool", lambda e, k=k: e.tensor_tensor(out=tq[k][:], in0=tq[k][:], in1=xb_[k][:], op=ALU.add),
                     reads=[B_tq[k], B_xb[k]], writes=[B_tq[k]])
                P.dma("sp", lambda e, t=t, j=j, k=k: e.dma_start(
                    out=x2_d[128 * t:128 * (t + 1), 512 * j:512 * (j + 1)], in_=tq[k][:]), B_tq[k], reads=[B_tq[k]])
        P.end()
    if stage <= 6:
        return nc

    P.begin()
    with ExitStack() as ph:
        def sbp(name, shape, dt):
            return ph.enter_context(nc.sbuf_tensor(name, list(shape), dt))

        def psp(name, shape, dt):
            return ph.enter_context(nc.psum_tensor(name, list(shape), dt))
        bc2 = sbp("bc2", [128, 2, 2, D], F32); B_bc2 = Buf("bc2")
        for g in range(2):
            for k in range(2):
                P.dma("sp", lambda e, g=g, k=k: e.dma_start(out=bc2[:, g, k, :],
                                                            in_=modrows[g, 3 + k, :].partition_broadcast(128)),
                      B_bc2, writes=[B_bc2])
        wr = sbp("wr", [128, KC, 36], F32); B_wr = Buf("wr")
        P.dma("sp", lambda e: e.dma_start(out=wr[:], in_=w_r.rearrange("(c p) n -> p c n", p=128)), B_wr, writes=[B_wr])
        brb = sbp("brb", [128, 36], F32); B_brb = Buf("brb")
        P.dma("sp", lambda e: e.dma_start(out=brb[:], in_=b_r[0, :].partition_broadcast(128)), B_brb, writes=[B_brb])
        valid = sbp("valid", [128, 1], F32); B_valid = Buf("valid")
        P.op("pool", lambda e: e.memset(valid[:], 1.0), writes=[B_valid])
        P.op("pool", lambda e: e.affine_select(out=valid[:], in_=valid[:], pattern=[[0, 1]], compare_op=ALU.is_ge,
                                               fill=0.0, base=SD - 1, channel_multiplier=-1), reads=[B_valid], writes=[B_valid])
        x2t = [sbp(f"x2t{i}", [128, D], F32) for i in range(2)]
        B_x2t = [Buf(f"x2t{i}") for i in range(2)]
        junk = sbp("junk2", [128, D], F32); B_junk = Buf("junk2")
        st = [sbp(f"st2_{i}", [128, 4], F32) for i in range(2)]
        B_st = [Buf(f"st2{i}") for i in range(2)]
        tmp = [sbp(f"tmp2_{i}", [128, D], F32) for i in range(2)]
        B_tmp = [Buf(f"tmp2{i}") for i in range(2)]
        h2b = [sbp(f"h2b{i}", [128, D], BF16) for i in range(2)]
        B_h2b = [Buf(f"h2b{i}") for i in range(2)]
        h2T = [sbp(f"h2T{i}", [128, KC, 128], F32) for i in range(2)]
        B_h2T = [Buf(f"h2T{i}") for i in range(2)]
        ptf = [psp(f"ptf{i}", [128, 4, 128], F32) for i in range(2)]
        B_ptf = [Buf(f"ptf{i}", True) for i in range(2)]
        plg = [psp(f"plg{i}", [128, 36], F32) for i in range(2)]
        B_plg = [Buf(f"plg{i}", True) for i in range(2)]
        rt = [sbp(f"rt{i}", [128, 96], F32) for i in range(2)]
        B_rt = [Buf(f"rt{i}") for i in range(2)]
        npt = 0
        for t in range(NT):
            i = t % 2
            g = 0 if t < 16 else 1
            P.dma("sp", lambda e, t=t, i=i: e.dma_start(out=x2t[i][:], in_=x2_d[128 * t:128 * (t + 1), :]),
                  B_x2t[i], writes=[B_x2t[i]])
            P.op("act", lambda e, i=i: e.activation(out=junk[:], in_=x2t[i][:], func=AF.Square, accum_out=st[i][:, 0:1]),
                 reads=[B_x2t[i]], writes=[B_junk, B_st[i]])
            P.op("dve", lambda e, i=i: e.tensor_scalar(out=st[i][:, 1:2], in0=st[i][:, 0:1], scalar1=1.0 / D, scalar2=EPS,
                                                       op0=ALU.mult, op1=ALU.add), reads=[B_st[i]], writes=[B_st[i]])
            P.op("act", lambda e, i=i: e.activation(out=st[i][:, 2:3], in_=st[i][:, 1:2], func=AF.Sqrt),
                 reads=[B_st[i]], writes=[B_st[i]])
            P.op("dve", lambda e, i=i: e.reciprocal(out=st[i][:, 3:4], in_=st[i][:, 2:3]), reads=[B_st[i]], writes=[B_st[i]])
            P.op("dve", lambda e, i=i, g=g: e.scalar_tensor_tensor(
                out=tmp[i][:], in0=x2t[i][:], scalar=st[i][:, 3:4], in1=bc2[:, g, 0, :], op0=ALU.mult, op1=ALU.mult),
                reads=[B_x2t[i], B_st[i], B_bc2], writes=[B_tmp[i]])
            P.op("pool", lambda e, i=i, g=g: e.tensor_tensor(out=tmp[i][:], in0=tmp[i][:], in1=bc2[:, g, 1, :], op=ALU.add),
                 reads=[B_tmp[i], B_bc2], writes=[B_tmp[i]])
            P.op("act", lambda e, i=i: e.copy(out=h2b[i][:], in_=tmp[i][:]), reads=[B_tmp[i]], writes=[B_h2b[i]])
            P.dma("sp", lambda e, t=t, i=i: e.dma_start(out=h2_d[128 * t:128 * (t + 1), :], in_=h2b[i][:]),
                  B_h2b[i], reads=[B_h2b[i]])
            for q4 in range(4):
                pi = npt % 2; npt += 1
                P.op("pe", [lambda e, i=i, c=c, pi=pi: e.transpose(out=ptf[pi][:, c % 4, :], in_=tmp[i][:, 128 * c:128 * (c + 1)],
                                                                 identity=ident_f[:]) for c in range(4 * q4, 4 * q4 + 4)],
                     reads=[B_tmp[i], B_ident], writes=[B_ptf[pi]])
                P.op("dve", lambda e, i=i, q4=q4, pi=pi: e.tensor_copy(out=h2T[i][:, 4 * q4:4 * q4 + 4, :], in_=ptf[pi][:]),
                     reads=[B_ptf[pi]], writes=[B_h2T[i]])
            P.op("pe", [lambda e, i=i, c=c: e.matmul(out=plg[i][:], lhsT=h2T[i][:, c, :], rhs=wr[:, c, :],
                                                      start=(c == 0), stop=(c == KC - 1)) for c in range(KC)],
                 reads=[B_h2T[i], B_wr], writes=[B_plg[i]])
            r = rt[i]; Br = B_rt[i]
            lg = r[:, 0:36]; gmax = r[:, 36:37]; ngmax = r[:, 37:38]; eg = r[:, 38:42]; gsum = r[:, 42:43]
            pg = r[:, 43:44]; ohg = r[:, 44:48]; pen = r[:, 48:52]; mx8 = r[:, 52:60]; nl1 = r[:, 60:61]
            rr = r[:, 61:62]; dn = r[:, 62:63]; rc = r[:, 63:64]
            el = sbp(f"el_{t}", [128, 4, 8], F32) if t < 2 else None
            if t < 2:
                els = getattr(P, "_els", []); els.append(el); P._els = els
            el = P._els[i]
            B_el = Br
            P.op("dve", lambda e, i=i, lg=lg: e.tensor_tensor(out=lg, in0=plg[i][:], in1=brb[:], op=ALU.add),
                 reads=[B_plg[i], B_brb], writes=[Br])
            P.op("dve", lambda e, lg=lg, gmax=gmax: e.reduce_max(out=gmax, in_=lg[:, 0:4], axis=AX.X), reads=[Br], writes=[Br])
            P.op("dve", lambda e, gmax=gmax, ngmax=ngmax: e.tensor_scalar(out=ngmax, in0=gmax, scalar1=-1.0, scalar2=None,
                                                                         op0=ALU.mult), reads=[Br], writes=[Br])
            P.op("act", lambda e, lg=lg, eg=eg, ngmax=ngmax, gsum=gsum: e.activation(
                out=eg, in_=lg[:, 0:4], func=AF.Exp, bias=ngmax, scale=1.0, accum_out=gsum), reads=[Br], writes=[Br])
            P.op("dve", lambda e, pg=pg, gsum=gsum: e.reciprocal(out=pg, in_=gsum), reads=[Br], writes=[Br])
            P.op("dve", lambda e, ohg=ohg, lg=lg, gmax=gmax: e.tensor_scalar(out=ohg, in0=lg[:, 0:4], scalar1=gmax, scalar2=None,
                                                                            op0=ALU.is_equal), reads=[Br], writes=[Br])
            P.op("dve", lambda e, ohg=ohg, pen=pen: e.tensor_scalar(out=pen, in0=ohg, scalar1=1e30, scalar2=-1e30,
                                                                    op0=ALU.mult, op1=ALU.add), reads=[Br], writes=[Br])
            P.op("dve", lambda e, el=el, lg=lg, pen=pen: e.tensor_tensor(
                out=el[:], in0=lg[:, 4:36].rearrange("p (g x) -> p g x", g=4),
                in1=pen.unsqueeze(2).to_broadcast([128, 4, 8]), op=ALU.add), reads=[Br], writes=[Br])
            elf = el[:].rearrange("p g x -> p (g x)")
            P.op("dve", lambda e, mx8=mx8, elf=elf: e.max(out=mx8, in_=elf), reads=[Br], writes=[Br])
            P.op("dve", lambda e, mx8=mx8, nl1=nl1: e.tensor_scalar(out=nl1, in0=mx8[:, 0:1], scalar1=-1.0, scalar2=None,
                                                                    op0=ALU.mult), reads=[Br], writes=[Br])
            P.op("act", lambda e, mx8=mx8, nl1=nl1, rr=rr: e.activation(out=rr, in_=mx8[:, 1:2], func=AF.Exp, bias=nl1, scale=1.0),
                 reads=[Br], writes=[Br])
            P.op("dve", lambda e, rr=rr, dn=dn: e.tensor_scalar(out=dn, in0=rr, scalar1=1.0, scalar2=None, op0=ALU.add),
                 reads=[Br], writes=[Br])
            P.op("dve", lambda e, dn=dn, rc=rc: e.reciprocal(out=rc, in_=dn), reads=[Br], writes=[Br])
            P.op("dve", lambda e, t=t, rc=rc, pg=pg: e.tensor_tensor(out=wgt_all[:, t, 0:1], in0=rc, in1=pg, op=ALU.mult),
                 reads=[Br, B_wgt], writes=[B_wgt])
            P.op("dve", lambda e, t=t, rr=rr: e.tensor_tensor(out=wgt_all[:, t, 1:2], in0=wgt_all[:, t, 0:1], in1=rr, op=ALU.mult),
                 reads=[Br, B_wgt], writes=[B_wgt])
            for k in range(2):
                P.op("dve", lambda e, t=t, k=k, elf=elf, mx8=mx8: e.tensor_scalar(
                    out=Mk_all[:, t, k, :], in0=elf, scalar1=mx8[:, k:k + 1], scalar2=None, op0=ALU.is_equal),
                    reads=[Br, B_Mk], writes=[B_Mk])
            if t == 16:
                P.op("dve", lambda e, t=t: e.tensor_scalar(out=Mk_all[:, t, :, :], in0=Mk_all[:, t, :, :], scalar1=valid[:, 0:1],
                                                           scalar2=None, op0=ALU.mult), reads=[B_Mk, B_valid], writes=[B_Mk])
        P.end()
    if stage <= 7:
        return nc

    P.begin()
    with ExitStack() as ph:
        def sbp(name, shape, dt):
            return ph.enter_context(nc.sbuf_tensor(name, list(shape), dt))

        def psp(name, shape, dt):
            return ph.enter_context(nc.psum_tensor(name, list(shape), dt))
        U = sbp("U", [128, 128], F32); ones_f = sbp("ones6", [128, 128], F32); B_c6 = Buf("c6")
        P.op("pool", lambda e: e.memset(U[:], 1.0), writes=[B_c6])
        P.op("pool", lambda e: e.affine_select(out=U[:], in_=U[:], pattern=[[1, 128]], compare_op=ALU.is_ge, fill=0.0,
                                               base=-1, channel_multiplier=-1), reads=[B_c6], writes=[B_c6])
        P.op("pool", lambda e: e.memset(ones_f[:], 1.0), reads=[B_c6], writes=[B_c6])
        pidx = sbp("pidx", [128, 4], F32); B_pidx = Buf("pidx")
        P.op("pool", lambda e: e.iota(pidx[:, 0:1], pattern=[[0, 1]], base=0, channel_multiplier=1,
                                      allow_small_or_imprecise_dtypes=True), writes=[B_pidx])
        P.op("dve", lambda e: e.tensor_scalar(out=pidx[:, 1:2], in0=pidx[:, 0:1], scalar1=2.0, scalar2=None, op0=ALU.mult),
             reads=[B_pidx], writes=[B_pidx])
        P.op("dve", lambda e: e.tensor_scalar(out=pidx[:, 2:3], in0=pidx[:, 0:1], scalar1=2.0, scalar2=1.0, op0=ALU.mult,
                                              op1=ALU.add), reads=[B_pidx], writes=[B_pidx])
        P.op("dve", lambda e: e.tensor_scalar(out=pidx[:, 3:4], in0=pidx[:, 0:1], scalar1=float(SD), scalar2=None,
                                              op0=ALU.is_ge), reads=[B_pidx], writes=[B_pidx])
        tmpi = sbp("tmpi", [128, 1], F32)
        P.op("dve", lambda e: e.tensor_scalar(out=tmpi[:], in0=pidx[:, 0:1], scalar1=float(NSLOT), scalar2=None, op0=ALU.add),
             reads=[B_pidx], writes=[B_pidx])
        P.op("dve", lambda e: e.tensor_tensor(out=pidx[:, 3:4], in0=pidx[:, 3:4], in1=tmpi[:], op=ALU.mult),
             reads=[B_pidx], writes=[B_pidx])
        Ms = sbp("Ms", [128, NT, 32], F32); B_Ms = Buf("Ms")
        P.op("dve", lambda e: e.tensor_tensor(out=Ms[:], in0=Mk_all[:, :, 0, :], in1=Mk_all[:, :, 1, :], op=ALU.add),
             reads=[B_Mk], writes=[B_Ms])
        Rall = sbp("Rall", [128, NT, 32], F32); B_R = Buf("Rall")
        pr = [psp(f"pr{i}", [128, 32], F32) for i in range(2)]
        B_pr = [Buf(f"pr{i}", True) for i in range(2)]
        for t in range(NT):
            i = t % 2
            fl = [lambda e, i=i, t2=t2: e.matmul(out=pr[i][:], lhsT=ones_f[:], rhs=Ms[:, t2, :], start=(t2 == 0), stop=False)
                  for t2 in range(t)]
            fl.append(lambda e, i=i, t=t: e.matmul(out=pr[i][:], lhsT=U[:], rhs=Ms[:, t, :], start=(t == 0), stop=True))
            P.op("pe", fl, reads=[B_Ms, B_c6], writes=[B_pr[i]])
            P.op("dve", lambda e, i=i, t=t: e.tensor_copy(out=Rall[:, t, :], in_=pr[i][:]), reads=[B_pr[i]], writes=[B_R])
        pc = psp("pc", [128, 32], F32); B_pc = Buf("pc", True)
        P.op("pe", [lambda e, t2=t2: e.matmul(out=pc[:], lhsT=ones_f[:], rhs=Ms[:, t2, :], start=(t2 == 0), stop=(t2 == NT - 1))
                    for t2 in range(NT)], reads=[B_Ms, B_c6], writes=[B_pc])
        cw = sbp("cw", [128, 8, 32], F32); B_cw = Buf("cw")
        ci = sbp("ci", [128, 2, 32], I32)
        P.op("dve", lambda e: e.tensor_scalar(out=cw[:, 0, :], in0=pc[:], scalar1=127.0, scalar2=None, op0=ALU.add),
             reads=[B_pc], writes=[B_cw])
        P.op("dve", lambda e: e.tensor_copy(out=ci[:, 0, :], in_=cw[:, 0, :]), reads=[B_cw], writes=[B_cw])
        P.op("dve", lambda e: e.tensor_scalar(out=ci[:, 1, :], in0=ci[:, 0, :], scalar1=7, scalar2=7,
                                              op0=ALU.arith_shift_right, op1=ALU.logical_shift_left), reads=[B_cw], writes=[B_cw])
        P.op("dve", lambda e: e.tensor_copy(out=cw[:, 1, :], in_=ci[:, 1, :]), reads=[B_cw], writes=[B_cw])
        P.op("pool", lambda e: e.memset(cw[:, 4, :], 1.0), reads=[B_cw], writes=[B_cw])
        P.op("dve", lambda e: e.tensor_tensor_scan(out=cw[:, 2, :], data0=cw[:, 4, :], data1=cw[:, 1, :], initial=0.0,
                                                   op0=ALU.mult, op1=ALU.add), reads=[B_cw], writes=[B_cw])
        P.op("dve", lambda e: e.tensor_tensor(out=cw[:, 3, :], in0=cw[:, 2, :], in1=cw[:, 1, :], op=ALU.subtract),
             reads=[B_cw], writes=[B_cw])
        basep = sbp("basep", [128, NT, 32], F32); prod = sbp("prod", [128, NT, 2, 32], F32)
        destf = sbp("destf", [128, NT, 2], F32); B_d = Buf("dwork")
        P.op("dve", lambda e: e.tensor_tensor(out=basep[:], in0=Rall[:], in1=cw[:, 3, :].unsqueeze(1).to_broadcast([128, NT, 32]),
                                              op=ALU.add), reads=[B_R, B_cw], writes=[B_d])
        for k in range(2):
            P.op("dve", lambda e, k=k: e.tensor_tensor(out=prod[:, :, k, :], in0=Mk_all[:, :, k, :], in1=basep[:], op=ALU.mult),
                 reads=[B_d, B_Mk], writes=[B_d])
        P.op("dve", lambda e: e.reduce_sum(out=destf[:].rearrange("p t k -> p (t k)"),
                                           in_=prod[:].rearrange("p t k x -> p (t k) x"), axis=AX.X), reads=[B_d], writes=[B_d])
        P.op("dve", lambda e: e.tensor_scalar(out=destf[:, 16, :], in0=destf[:, 16, :], scalar1=pidx[:, 3:4], scalar2=None,
                                              op0=ALU.add), reads=[B_d, B_pidx], writes=[B_d])
        P.op("dve", lambda e: e.tensor_copy(out=desti[:], in_=destf[:]), reads=[B_d], writes=[B_desti])
        thr = sbp("thr", [128, NBLK], F32)
        P.op("pool", lambda e: e.iota(thr[:], pattern=[[128, NBLK]], base=0, channel_multiplier=0,
                                      allow_small_or_imprecise_dtypes=True), writes=[B_d])
        cmp = sbp("cmp", [128, NBLK, 32], F32); blke = sbp("blke", [128, NBLK], F32); wf = sbp("wf", [128, NBLK, 2], F32)
        P.op("dve", lambda e: e.tensor_tensor(out=cmp[:], in0=cw[:, 2, :].unsqueeze(1).to_broadcast([128, NBLK, 32]),
                                              in1=thr[:].unsqueeze(2).to_broadcast([128, NBLK, 32]), op=ALU.is_le),
             reads=[B_cw, B_d], writes=[B_d])
        P.op("dve", lambda e: e.reduce_sum(out=blke[:], in_=cmp[:], axis=AX.X), reads=[B_d], writes=[B_d])
        same = sbp("same", [128, NBLK], F32)
        P.op("pool", lambda e: e.memset(same[:], 0.0), reads=[B_d], writes=[B_d])
        P.op("dve", lambda e: e.tensor_tensor(out=same[:, 1:NBLK], in0=blke[:, 1:NBLK], in1=blke[:, 0:NBLK - 1], op=ALU.is_equal),
             reads=[B_d], writes=[B_d])
        P.op("dve", lambda e: e.scalar_tensor_tensor(out=blke[:], in0=same[:], scalar=40.0, in1=blke[:], op0=ALU.mult, op1=ALU.add),
             reads=[B_d], writes=[B_d])
        P.op("dve", lambda e: e.tensor_scalar(out=blke[:], in0=blke[:], scalar1=256.0, scalar2=None, op0=ALU.mult),
             reads=[B_d], writes=[B_d])
        for hh in range(2):
            P.op("dve", lambda e, hh=hh: e.tensor_scalar(out=wf[:, :, hh], in0=blke[:], scalar1=pidx[:, 1 + hh:2 + hh],
                                                         scalar2=None, op0=ALU.add), reads=[B_d, B_pidx], writes=[B_d])
        P.op("dve", lambda e: e.tensor_copy(out=widx[:], in_=wf[:]), reads=[B_d], writes=[B_widx])
        B_xbd = Buf("xbd")
        zrow = sbp("zrow", [128, D], BF16); B_zrow = Buf("zrow")
        P.op("pool", lambda e: e.memset(zrow[:], 0.0), writes=[B_zrow])
        for bb in range(NBLK + 1):
            P.dma("sp", lambda e, bb=bb: e.dma_start(out=xb_d[128 * bb:128 * (bb + 1), :], in_=zrow[:]), B_zrow,
                  reads=[B_zrow], writes=[B_xbd])
        hrow = [sbp(f"hrow{i}", [128, D], BF16) for i in range(2)]
        B_hrow = [Buf(f"hrow{i}") for i in range(2)]
        for t in range(NT):
            i = t % 2
            P.dma("sp", lambda e, t=t, i=i: e.dma_start(out=hrow[i][:], in_=h2_d[128 * t:128 * (t + 1), :]),
                  B_hrow[i], writes=[B_hrow[i]])
            for k in range(2):
                P.dma("pool", lambda e, t=t, i=i, k=k: e.indirect_dma_start(
                    out=xb_d[:, :], out_offset=bass.IndirectOffsetOnAxis(ap=desti[:, t, k:k + 1], axis=0),
                    in_=hrow[i][:], in_offset=None), B_hrow[i], reads=[B_hrow[i], B_desti], writes=[B_xbd])
        P.end()
    if stage <= 8:
        return nc

    P.begin()
    with ExitStack() as ph:
        def sbp(name, shape, dt):
            return ph.enter_context(nc.sbuf_tensor(name, list(shape), dt))

        def psp(name, shape, dt):
            return ph.enter_context(nc.psum_tensor(name, list(shape), dt))
        NWS = 6
        ws = [sbp(f"ws{i}", [128, 8192], BF16) for i in range(NWS)]
        B_ws = [Buf(f"ws{i}") for i in range(NWS)]
        Xb = [sbp(f"Xb{i}", [128, D], BF16) for i in range(2)]; B_Xb = [Buf(f"Xb{i}") for i in range(2)]
        XT = [sbp(f"XT{i}", [128, KC, 128], BF16) for i in range(2)]; B_XT = [Buf(f"XT{i}") for i in range(2)]
        sg = [sbp(f"sg{i}", [128, 512], F32) for i in range(2)]; B_sg = [Buf(f"sg{i}") for i in range(2)]
        Hb = [sbp(f"Hb{i}", [128, 1024], BF16) for i in range(2)]; B_Hb = [Buf(f"Hb{i}") for i in range(2)]
        HT = [sbp(f"HT{i}", [128, 8, 128], BF16) for i in range(2)]; B_HT = [Buf(f"HT{i}") for i in range(2)]
        ysb = [sbp(f"ysb{i}", [128, D], F32) for i in range(2)]; B_ysb = [Buf(f"ysb{i}") for i in range(2)]
        pG = [psp(f"pG{i}", [128, 512], F32) for i in range(2)]; pU = [psp(f"pU{i}", [128, 512], F32) for i in range(2)]
        pY = [psp(f"pY{i}", [128, 512], F32) for i in range(2)]
        B_pG = [Buf(f"pG{i}", True) for i in range(2)]; B_pU = [Buf(f"pU{i}", True) for i in range(2)]
        B_pY = [Buf(f"pY{i}", True) for i in range(2)]
        ptx = [psp(f"ptx{i}", [128, 4, 128], BF16) for i in range(2)]; B_ptx = [Buf(f"ptx{i}", True) for i in range(2)]
        _bcr = {}

        def bc_reg(e):
            if "r" not in _bcr:
                _bcr["r"] = e.to_reg(8191)
            return _bcr["r"]
        wsrc = {"g": w_eg.rearrange("(r c) n -> r (c n)", c=8), "u": w_eu.rearrange("(r c) n -> r (c n)", c=8),
                "d": w_ed.rearrange("(r c) n -> r (c n)", c=4)}
        nws = 0; npt = 0
        zr7 = sbp("zr7", [128, D], F32); B_zr7 = Buf("zr7")
        P.op("pool", lambda e: e.memset(zr7[:], 0.0), writes=[B_zr7])
        P.dma("sp", lambda e: e.dma_start(out=yb_d[NSLOT:NSLOT + 128, :], in_=zr7[:]), B_zr7, reads=[B_zr7])
        for b in range(NBLK):
            i = b % 2
            P.dma("sp", lambda e, b=b, i=i: e.dma_start(out=Xb[i][:], in_=xb_d[128 * b:128 * (b + 1), :]), B_Xb[i], writes=[B_Xb[i]])
            slots = {}
            for mat in ("g", "u", "d"):
                for hh in range(2):
                    sl = nws % NWS; nws += 1
                    slots[(mat, hh)] = sl
                    P.dma("pool", lambda e, b=b, mat=mat, hh=hh, sl=sl: e.indirect_dma_start(
                        out=ws[sl][:], out_offset=None, in_=wsrc[mat],
                        in_offset=bass.IndirectOffsetOnAxis(ap=widx[:, b, hh:hh + 1], axis=0),
                        bounds_check=bc_reg(e), oob_is_err=False),
                        B_ws[sl], reads=[B_widx], writes=[B_ws[sl]])
            for q4 in range(4):
                pi = npt % 2; npt += 1
                P.op("pe", [lambda e, i=i, j=j, pi=pi: e.transpose(out=ptx[pi][:, j % 4, :], in_=Xb[i][:, j:D:16],
                                                                 identity=ident_b[:]) for j in range(4 * q4, 4 * q4 + 4)],
                     reads=[B_Xb[i], B_ident], writes=[B_ptx[pi]])
                if q4 % 2 == 0:
                    P.op("act", lambda e, i=i, q4=q4, pi=pi: e.copy(out=XT[i][:, 4 * q4:4 * q4 + 4, :], in_=ptx[pi][:]),
                         reads=[B_ptx[pi]], writes=[B_XT[i]])
                else:
                    P.op("dve", lambda e, i=i, q4=q4, pi=pi: e.tensor_copy(out=XT[i][:, 4 * q4:4 * q4 + 4, :], in_=ptx[pi][:]),
                         reads=[B_ptx[pi]], writes=[B_XT[i]])
            for mat, pp, Bp in (("g", pG, B_pG), ("u", pU, B_pU)):
                for hh in range(2):
                    sl = slots[(mat, hh)]
                    for fh in range(2):
                        P.op("pe", [lambda e, i=i, c=c, hh=hh, fh=fh, sl=sl, pp=pp: e.matmul(
                            out=pp[fh][:], lhsT=XT[i][:, 8 * hh + c, :], rhs=ws[sl][:, 1024 * c + 512 * fh:1024 * c + 512 * fh + 512],
                            start=(hh == 0 and c == 0), stop=(hh == 1 and c == 7)) for c in range(8)],
                            reads=[B_XT[i], B_ws[sl]], writes=[Bp[fh]])
            for fh in range(2):
                P.op("act", lambda e, fh=fh: e.activation(out=sg[fh][:], in_=pG[fh][:], func=AF.Silu),
                     reads=[B_pG[fh]], writes=[B_sg[fh]])
                P.op("dve", lambda e, i=i, fh=fh: e.tensor_tensor(out=Hb[i][:, 512 * fh:512 * (fh + 1)], in0=sg[fh][:],
                                                                  in1=pU[fh][:], op=ALU.mult),
                     reads=[B_sg[fh], B_pU[fh]], writes=[B_Hb[i]])
            for q4 in range(2):
                pi = npt % 2; npt += 1
                P.op("pe", [lambda e, i=i, j=j, pi=pi: e.transpose(out=ptx[pi][:, j % 4, :], in_=Hb[i][:, j:1024:8],
                                                                 identity=ident_b[:]) for j in range(4 * q4, 4 * q4 + 4)],
                     reads=[B_Hb[i], B_ident], writes=[B_ptx[pi]])
                P.op("act", lambda e, i=i, q4=q4, pi=pi: e.copy(out=HT[i][:, 4 * q4:4 * q4 + 4, :], in_=ptx[pi][:]),
                     reads=[B_ptx[pi]], writes=[B_HT[i]])
            for dh in range(2):
                for q in range(2):
                    fl = []
                    for hh in range(2):
                        sl = slots[("d", hh)]
                        for c in range(4):
                            fl.append(lambda e, i=i, c=c, hh=hh, sl=sl, q=q, dh=dh: e.matmul(
                                out=pY[q][:], lhsT=HT[i][:, 4 * hh + c, :],
                                rhs=ws[sl][:, 2048 * c + 1024 * dh + 512 * q:2048 * c + 1024 * dh + 512 * q + 512],
                                start=(hh == 0 and c == 0), stop=(hh == 1 and c == 3)))
                    P.op("pe", fl, reads=[B_HT[i], B_ws[slots[("d", 0)]], B_ws[slots[("d", 1)]]], writes=[B_pY[q]])
                    col = 1024 * dh + 512 * q
                    if q == 0:
                        P.op("act", lambda e, i=i, q=q, col=col: e.copy(out=ysb[i][:, col:col + 512], in_=pY[q][:]),
                             reads=[B_pY[q]], writes=[B_ysb[i]])
                    else:
                        P.op("dve", lambda e, i=i, q=q, col=col: e.tensor_copy(out=ysb[i][:, col:col + 512], in_=pY[q][:]),
                             reads=[B_pY[q]], writes=[B_ysb[i]])
            P.dma("sp", lambda e, b=b, i=i: e.dma_start(out=yb_d[128 * b:128 * (b + 1), :], in_=ysb[i][:]),
                  B_ysb[i], reads=[B_ysb[i]])
        P.end()
    if stage <= 9:
        return nc

    P.begin()
    with ExitStack() as ph:
        def sbp(name, shape, dt):
            return ph.enter_context(nc.sbuf_tensor(name, list(shape), dt))
        bc3 = sbp("bc3", [128, 3, D], F32); B_bc3 = Buf("bc3")
        for g in range(2):
            P.dma("sp", lambda e, g=g: e.dma_start(out=bc3[:, g, :], in_=modrows[g, 5, :].partition_broadcast(128)),
                  B_bc3, writes=[B_bc3])
        P.dma("sp", lambda e: e.dma_start(out=bc3[:, 2, :], in_=g_fin[0, :].partition_broadcast(128)), B_bc3, writes=[B_bc3])
        y0 = [sbp(f"y0_{i}", [128, D], F32) for i in range(2)]; y1 = [sbp(f"y1_{i}", [128, D], F32) for i in range(2)]
        xx = [sbp(f"xx_{i}", [128, D], F32) for i in range(2)]
        B_y0 = [Buf(f"y0{i}") for i in range(2)]; B_y1 = [Buf(f"y1{i}") for i in range(2)]; B_xx = [Buf(f"xx{i}") for i in range(2)]
        junk = sbp("junk3", [128, D], F32); B_junk = Buf("junk3")
        st = [sbp(f"st3_{i}", [128, 4], F32) for i in range(2)]; B_st = [Buf(f"st3{i}") for i in range(2)]
        for t in range(NT):
            i = t % 2
            g = 0 if t < 16 else 1
            P.dma("pool", lambda e, t=t, i=i: e.indirect_dma_start(
                out=y0[i][:], out_offset=None, in_=yb_d[:, :],
                in_offset=bass.IndirectOffsetOnAxis(ap=desti[:, t, 0:1], axis=0)), B_y0[i], reads=[B_desti], writes=[B_y0[i]])
            P.dma("pool", lambda e, t=t, i=i: e.indirect_dma_start(
                out=y1[i][:], out_offset=None, in_=yb_d[:, :],
                in_offset=bass.IndirectOffsetOnAxis(ap=desti[:, t, 1:2], axis=0)), B_y1[i], reads=[B_desti], writes=[B_y1[i]])
            P.dma("sp", lambda e, t=t, i=i: e.dma_start(out=xx[i][:], in_=x2_d[128 * t:128 * (t + 1), :]), B_xx[i], writes=[B_xx[i]])
            P.op("dve", lambda e, t=t, i=i: e.tensor_scalar(out=y0[i][:], in0=y0[i][:], scalar1=wgt_all[:, t, 0:1], scalar2=None,
                                                            op0=ALU.mult), reads=[B_y0[i], B_wgt], writes=[B_y0[i]])
            P.op("dve", lambda e, t=t, i=i: e.scalar_tensor_tensor(out=y1[i][:], in0=y1[i][:], scalar=wgt_all[:, t, 1:2],
                                                                   in1=y0[i][:], op0=ALU.mult, op1=ALU.add),
                 reads=[B_y0[i], B_y1[i], B_wgt], writes=[B_y1[i]])
            P.op("pool", lambda e, i=i, g=g: e.tensor_tensor(out=y1[i][:], in0=y1[i][:], in1=bc3[:, g, :], op=ALU.mult),
                 reads=[B_y1[i], B_bc3], writes=[B_y1[i]])
            P.op("dve", lambda e, i=i: e.tensor_tensor(out=xx[i][:], in0=xx[i][:], in1=y1[i][:], op=ALU.add),
                 reads=[B_xx[i], B_y1[i]], writes=[B_xx[i]])
            P.op("act", lambda e, i=i: e.activation(out=junk[:], in_=xx[i][:], func=AF.Square, accum_out=st[i][:, 0:1]),
                 reads=[B_xx[i]], writes=[B_junk, B_st[i]])
            P.op("dve", lambda e, i=i: e.tensor_scalar(out=st[i][:, 1:2], in0=st[i][:, 0:1], scalar1=1.0 / D, scalar2=EPS,
                                                       op0=ALU.mult, op1=ALU.add), reads=[B_st[i]], writes=[B_st[i]])
            P.op("act", lambda e, i=i: e.activation(out=st[i][:, 2:3], in_=st[i][:, 1:2], func=AF.Sqrt),
                 reads=[B_st[i]], writes=[B_st[i]])
            P.op("dve", lambda e, i=i: e.reciprocal(out=st[i][:, 3:4], in_=st[i][:, 2:3]), reads=[B_st[i]], writes=[B_st[i]])
            P.op("dve", lambda e, i=i: e.scalar_tensor_tensor(out=y0[i][:], in0=xx[i][:], scalar=st[i][:, 3:4], in1=bc3[:, 2, :],
                                                              op0=ALU.mult, op1=ALU.mult),
                 reads=[B_xx[i], B_st[i], B_bc3, B_y1[i]], writes=[B_y0[i]])
            if t < 16:
                P.dma("sp", lambda e, t=t, i=i: e.dma_start(out=y_p[128 * t:128 * (t + 1), :], in_=y0[i][:]), B_y0[i], reads=[B_y0[i]])
            else:
                P.dma("sp", lambda e, i=i: e.dma_start(out=y_s, in_=y0[i][0:SD, :]), B_y0[i], reads=[B_y0[i]])
        P.end()
    return nc


_NC = None


def kernel(**inp):
    global _NC
    f = lambda a: np.ascontiguousarray(a, dtype=np.float32)
    pos = np.arange(TOK, dtype=np.float32)
    pos[2048:2048 + SD] = 2048 + np.arange(SD)
    half = 64
    inv = np.exp(-math.log(10000.0) * np.arange(half, dtype=np.float32) / half).astype(np.float32)
    ang = pos[:, None] * inv[None, :]
    rope_cs = np.concatenate([np.cos(ang), np.sin(ang)], axis=1).astype(np.float32)
    shared = {
        "w_ada": f(inp["w_ada"][0]), "b_ada": f(inp["b_ada"][0][None]), "g_mix": f(inp["g_mix"][0][None]),
        "w_in": f(inp["w_in"][0]), "w_pa": f(inp["w_pa"][0]), "w_pb": f(inp["w_pb"][0]), "w_out": f(inp["w_out"][0]),
        "lamv": f(np.stack([inp["lam_q1"][0], inp["lam_k1"][0], inp["lam_q2"][0], inp["lam_k2"][0]])),
        "g_subln": f(inp["g_subln"][0].reshape(256, 1)), "g_moe": f(inp["g_moe"][0][None]),
        "w_r": f(np.concatenate([inp["w_rg"][0], inp["w_re"][0]], axis=1)),
        "b_r": f(np.concatenate([inp["b_rg"][0], inp["b_re"][0]])[None]),
        "w_eg": f(inp["w_e_gate"][0].reshape(32 * 2048, 1024)), "w_eu": f(inp["w_e_up"][0].reshape(32 * 2048, 1024)),
        "w_ed": f(inp["w_e_down"][0].reshape(32 * 1024, 2048)), "g_fin": f(inp["g_final"][None]),
        "rope_cs": rope_cs,
    }
    in_maps = []
    for b in range(8):
        m = dict(shared)
        m["x_p"] = f(inp["x_prompt"][b]); m["x_s"] = f(inp["x_sample"][b])
        m["c_sb_k"] = f(inp["cache_sb_k"][0, b].reshape(S, 1024)); m["c_sb_v"] = f(inp["cache_sb_v"][0, b].reshape(S, 1024))
        m["c_df_k"] = f(inp["cache_df_k"][0, b].reshape(S, 1024)); m["c_df_v"] = f(inp["cache_df_v"][0, b].reshape(S, 1024))
        m["c_in"] = f(np.stack([inp["c_prompt"][b], inp["c_sample"][b]]))
        in_maps.append(m)
    if _NC is None:
        _NC = build_program()
    res = run_bass_kernel_spmd(_NC, in_maps, core_ids=list(range(8)))
    R = res.results

    def g(name, shape):
        return np.stack([np.asarray(R[b][name], dtype=np.float32).reshape(shape) for b in range(8)])
    y_p = g("y_p", (S, D)); y_s = g("y_s", (SD, D))
    outs = [y_p, y_s,
            g("o_sbk_p", (S, 8, 128))[None], g("o_sbv_p", (S, 8, 128))[None],
            g("o_dfk_p", (S, 4, 2, 128))[None], g("o_dfv_p", (S, 4, 256))[None],
            g("o_sbk_s", (SD, 8, 128))[None], g("o_sbv_s", (SD, 8, 128))[None],
            g("o_dfk_s", (SD, 4, 2, 128))[None], g("o_dfv_s", (SD, 4, 256))[None]]
    return tuple(outs)
```

```python
import math
from contextlib import ExitStack
import numpy as np
import concourse.bass as bass
import concourse.mybir as mybir
from concourse.bass_utils import run_bass_kernel_spmd

F32 = mybir.dt.float32
BF16 = mybir.dt.bfloat16
I32 = mybir.dt.int32
AF = mybir.ActivationFunctionType
ALU = mybir.AluOpType
AX = mybir.AxisListType

D = 2048
S = 2048
SD = 16
NT = 17
TOK = NT * 128
KC = 16
INW = 10240
EPS = 1e-6
NBLK = 64
NSLOT = NBLK * 128
SCALE = 128 ** -0.5
LAM_INIT = 0.8 - 0.6 * math.exp(0.0)

STAGE = 99


class Buf:
    def __init__(self, name, excl=False):
        self.name = name
        self.excl = excl
        self.writers = []
        self.readers = []
        self.dsem = None
        self.dcnt = 0


class Eng:
    def __init__(self, key):
        self.key = key
        self.ops = []
        self.sem = None
        self.cnt = 0
        self.waited = {}


class Prog:
    def __init__(self, nc):
        self.nc = nc
        self.phase = 0
        self.es = None
        self.glob = ExitStack()

    def begin(self):
        self.es = ExitStack()
        self.eng = {k: Eng(k) for k in ("pe", "act", "dve", "pool", "sp")}
        for k, e in self.eng.items():
            e.sem = self.es.enter_context(self.nc.semaphore(f"ph{self.phase}_{k}"))
        self.dma_final = {}
        self.dmap = {}
        self.nxt = {"sw": 0, "hw": 0}

    def _waits(self, eng, reads, writes):
        toks = []
        for b in reads:
            toks += b.writers
            if b.excl:
                toks += b.readers
        for b in writes:
            toks += b.writers + b.readers
        out = {}
        for (s, v) in toks:
            if s is eng.sem and eng.key in ("pe", "sp"):
                continue
            if eng.waited.get(id(s), 0) >= v:
                continue
            if id(s) not in out or out[id(s)][1] < v:
                out[id(s)] = (s, v)
        for (s, v) in out.values():
            eng.waited[id(s)] = v
        return list(out.values())

    def _update(self, tok, reads, writes):
        for b in reads:
            b.readers.append(tok)
        for b in writes:
            b.writers = [tok]
            b.readers = []

    def op(self, ek, fns, reads=(), writes=()):
        eng = self.eng[ek]
        if not isinstance(fns, (list, tuple)):
            fns = [fns]
        waits = self._waits(eng, reads, writes)
        eng.cnt += 1
        tok = (eng.sem, eng.cnt)
        eng.ops.append((waits, list(fns), (eng.sem, 1)))
        self._update(tok, reads, writes)

    def dma(self, qk, fn, sembuf, reads=(), writes=()):
        eng = self.eng[qk]
        kind = "sw" if qk == "pool" else "hw"
        if not hasattr(self, "pools"):
            self.pools = {"sw": [self.glob.enter_context(self.nc.semaphore(f"swd{i}")) for i in range(14)],
                          "hw": [self.glob.enter_context(self.nc.semaphore(f"hwd{i}")) for i in range(20)]}
            self.semval = {}
        key = (id(sembuf), kind)
        if key not in self.dmap:
            idx = self.nxt[kind]; self.nxt[kind] += 1
            self.dmap[key] = self.pools[kind][idx]
        sem = self.dmap[key]
        waits = self._waits(eng, reads, writes)
        self.semval[id(sem)] = self.semval.get(id(sem), 0) + 16
        tok = (sem, self.semval[id(sem)])
        eng.ops.append((waits, [fn], (sem, 16)))
        self._update(tok, reads, writes)
        self.dma_final[id(sem)] = tok

    def end(self):
        nc = self.nc
        finals = list(self.dma_final.values())
        engs = self.eng

        def emit(e, eng, extra=()):
            for waits, fns, inc in eng.ops:
                for (s, v) in waits:
                    e.wait_ge(s, v)
                ins = None
                for f in fns:
                    ins = f(e)
                ins.then_inc(inc[0], inc[1])
            for (s, v) in extra:
                e.wait_ge(s, v)

        with nc.Block() as blk:
            @blk.sync
            def _(e):
                emit(e, engs["sp"], finals)

            @blk.tensor
            def _(e):
                emit(e, engs["pe"])

            @blk.scalar
            def _(e):
                emit(e, engs["act"])

            @blk.vector
            def _(e):
                emit(e, engs["dve"])

            @blk.gpsimd
            def _(e):
                emit(e, engs["pool"])
        self.es.close()
        self.phase += 1


def build_program(stage=None):
    stage = STAGE if stage is None else stage
    nc = bass.Bass("TRN2", target_bir_lowering=False)
    es = ExitStack()

    def din(name, shape, dt=F32):
        return nc.dram_tensor(name, list(shape), dt, kind="ExternalInput").ap()

    def dout(name, shape, dt=F32):
        return nc.dram_tensor(name, list(shape), dt, kind="ExternalOutput").ap()

    def dscr(name, shape, dt=F32):
        return nc.dram_tensor(name, list(shape), dt, kind="Internal").ap()

    x_p = din("x_p", [S, D]); x_s = din("x_s", [SD, D])
    c_sb_k = din("c_sb_k", [S, 1024]); c_sb_v = din("c_sb_v", [S, 1024])
    c_df_k = din("c_df_k", [S, 1024]); c_df_v = din("c_df_v", [S, 1024])
    c_in = din("c_in", [2, D])
    w_ada = din("w_ada", [D, 6 * D]); b_ada = din("b_ada", [1, 6 * D])
    g_mix = din("g_mix", [1, D]); w_in = din("w_in", [D, INW])
    w_pa = din("w_pa", [1024, D]); w_pb = din("w_pb", [1024, D]); w_out = din("w_out", [D, D])
    lamv = din("lamv", [4, 128]); g_subln = din("g_subln", [256, 1])
    g_moe = din("g_moe", [1, D]); w_r = din("w_r", [D, 36]); b_r = din("b_r", [1, 36])
    w_eg = din("w_eg", [32 * 2048, 1024]); w_eu = din("w_eu", [32 * 2048, 1024]); w_ed = din("w_ed", [32 * 1024, 2048])
    g_fin = din("g_fin", [1, D])
    rope_cs = din("rope_cs", [TOK, 128])

    y_p = dout("y_p", [S, D]); y_s = dout("y_s", [SD, D])
    o_sbk_p = dout("o_sbk_p", [S, 1024]); o_sbv_p = dout("o_sbv_p", [S, 1024])
    o_dfk_p = dout("o_dfk_p", [S, 1024]); o_dfv_p = dout("o_dfv_p", [S, 1024])
    o_sbk_s = dout("o_sbk_s", [SD, 1024]); o_sbv_s = dout("o_sbv_s", [SD, 1024])
    o_dfk_s = dout("o_dfk_s", [SD, 1024]); o_dfv_s = dout("o_dfv_s", [SD, 1024])

    modrows = dscr("modrows", [2, 6, D])
    qkT_d = dscr("qkT_d", [4, 8, 128, TOK], BF16)
    v_d = dscr("v_d", [2, TOK, 1024], BF16)
    gates_d = dscr("gates_d", [TOK, 4096], BF16)
    x2_d = dscr("x2_d", [TOK, D])
    h2_d = dscr("h2_d", [TOK, D], BF16)
    xb_d = dscr("xb_d", [NSLOT + 128, D], BF16)
    yb_d = dscr("yb_d", [NSLOT + 128, D])

    P = Prog(nc)

    def sb(name, shape, dt):
        return es.enter_context(nc.sbuf_tensor(name, list(shape), dt))

    ident_b = sb("ident_b", [128, 128], BF16)
    ident_f = sb("ident_f", [128, 128], F32)
    B_ident = Buf("ident")

    def tok_rows(t):
        return 128 if t < 16 else SD

    P.begin()
    with ExitStack() as ph:
        def sbp(name, shape, dt):
            return ph.enter_context(nc.sbuf_tensor(name, list(shape), dt))

        def psp(name, shape, dt):
            return ph.enter_context(nc.psum_tensor(name, list(shape), dt))

        P.op("pool", lambda e: e.memset(ident_b[:], 0.0), writes=[B_ident])
        P.op("pool", lambda e: e.affine_select(out=ident_b[:], in_=ident_b[:], pattern=[[-1, 128]],
                                               compare_op=ALU.not_equal, fill=1.0, base=0, channel_multiplier=1),
             reads=[B_ident], writes=[B_ident])
        P.op("pool", lambda e: e.tensor_copy(out=ident_f[:], in_=ident_b[:]), reads=[B_ident], writes=[B_ident])

        cT32 = sbp("cT32", [128, KC, 2], F32)
        cT = sbp("cT", [128, KC, 2], BF16)
        B_cT32 = Buf("cT32"); B_cT = Buf("cT")
        for g in range(2):
            P.dma("sp", lambda e, g=g: e.dma_start(out=cT32[:, :, g], in_=c_in[g, :].rearrange("(c p) -> p c", p=128),
                                                   allow_slow_non_contiguous=True), B_cT32, writes=[B_cT32])
        P.op("dve", lambda e: e.tensor_copy(out=cT[:], in_=cT32[:]), reads=[B_cT32], writes=[B_cT])

        modsb = sbp("modsb", [2, 6 * D], F32)
        bada = sbp("bada", [2, 6 * D], F32)
        gvec = sbp("gvec", [2, 2, D], F32)
        B_mod = Buf("modsb"); B_bada = Buf("bada"); B_gvec = Buf("gvec")
        for g in range(2):
            P.dma("sp", lambda e, g=g: e.dma_start(out=bada[g:g + 1, :], in_=b_ada), B_bada, writes=[B_bada])
            P.dma("sp", lambda e, g=g: e.dma_start(out=gvec[g:g + 1, 0, :], in_=g_mix), B_gvec, writes=[B_gvec])
            P.dma("sp", lambda e, g=g: e.dma_start(out=gvec[g:g + 1, 1, :], in_=g_moe), B_gvec, writes=[B_gvec])

        NW = 3
        wslots = [sbp(f"w0_{i}", [128, KC, 512], BF16) for i in range(NW)]
        B_w = [Buf(f"w0_{i}") for i in range(NW)]
        pm = [psp(f"pm{i}", [2, 512], F32) for i in range(2)]
        B_pm = [Buf(f"pm{i}") for i in range(2)]
        for j in range(24):
            s = j % NW
            P.dma("pool", lambda e, j=j, s=s: e.dma_start(
                out=wslots[s][:], in_=w_ada[:, 512 * j:512 * (j + 1)].rearrange("(c p) n -> p c n", p=128)),
                B_w[s], writes=[B_w[s]])
            pp = pm[j % 2]; bp = B_pm[j % 2]
            P.op("pe", [lambda e, c=c, s=s, pp=pp: e.matmul(out=pp[:], lhsT=cT[:, c, :], rhs=wslots[s][:, c, :],
                                                              start=(c == 0), stop=(c == KC - 1)) for c in range(KC)],
                 reads=[B_cT, B_w[s]], writes=[bp])
            P.op("dve", lambda e, j=j, pp=pp: e.tensor_tensor(out=modsb[:, 512 * j:512 * (j + 1)], in0=pp[:],
                                                              in1=bada[:, 512 * j:512 * (j + 1)], op=ALU.add),
                 reads=[bp, B_bada], writes=[B_mod])
        rows = bada[:].rearrange("p (k d) -> p k d", k=6)
        B_rows = B_bada

        def msl(k):
            return modsb[:, D * k:D * (k + 1)]
        P.op("dve", lambda e: e.scalar_tensor_tensor(out=rows[:, 0, :], in0=msl(1), scalar=1.0, in1=gvec[:, 0, :],
                                                     op0=ALU.add, op1=ALU.mult), reads=[B_mod, B_gvec], writes=[B_rows])
        P.op("dve", lambda e: e.tensor_copy(out=rows[:, 1, :], in_=msl(0)), reads=[B_mod], writes=[B_rows])
        P.op("dve", lambda e: e.tensor_copy(out=rows[:, 2, :], in_=msl(2)), reads=[B_mod], writes=[B_rows])
        P.op("dve", lambda e: e.scalar_tensor_tensor(out=rows[:, 3, :], in0=msl(4), scalar=1.0, in1=gvec[:, 1, :],
                                                     op0=ALU.add, op1=ALU.mult), reads=[B_mod, B_gvec], writes=[B_rows])
        P.op("dve", lambda e: e.tensor_copy(out=rows[:, 4, :], in_=msl(3)), reads=[B_mod], writes=[B_rows])
        P.op("dve", lambda e: e.tensor_copy(out=rows[:, 5, :], in_=msl(5)), reads=[B_mod], writes=[B_rows])
        P.dma("sp", lambda e: e.dma_start(out=modrows, in_=rows), B_rows, reads=[B_rows])
        P.end()
    if stage <= 0:
        return nc

    def bc_row(src_row):
        return src_row.broadcast(0, 128) if hasattr(src_row, "broadcast") else src_row

    with ExitStack() as outer:
        hT = outer.enter_context(nc.sbuf_tensor("hT", [128, KC, TOK], BF16))
        B_hT = [Buf(f"hT{t}") for t in range(NT)]

        P.begin()
        with ExitStack() as ph:
            def sbp(name, shape, dt):
                return ph.enter_context(nc.sbuf_tensor(name, list(shape), dt))

            def psp(name, shape, dt):
                return ph.enter_context(nc.psum_tensor(name, list(shape), dt))
            bc = sbp("bc1", [128, 2, 2, D], F32)
            B_bc = Buf("bc1")
            for g in range(2):
                for k in range(2):
                    P.dma("sp", lambda e, g=g, k=k: e.dma_start(
                        out=bc[:, g, k, :], in_=modrows[g, k, :].partition_broadcast(128)), B_bc, writes=[B_bc])
            xt = [sbp(f"xt{i}", [128, D], F32) for i in range(2)]
            B_xt = [Buf(f"xt{i}") for i in range(2)]
            junk = sbp("junk", [128, D], F32); B_junk = Buf("junk")
            st = [sbp(f"st{i}", [128, 4], F32) for i in range(2)]
            B_st = [Buf(f"st{i}") for i in range(2)]
            tmp = [sbp(f"tmp{i}", [128, D], F32) for i in range(2)]
            B_tmp = [Buf(f"tmp{i}") for i in range(2)]
            hb = [sbp(f"hb{i}", [128, D], BF16) for i in range(2)]
            B_hb = [Buf(f"hb{i}") for i in range(2)]
            pt = [psp(f"pt{i}", [128, 4, 128], BF16) for i in range(2)]
            B_pt = [Buf(f"pt{i}") for i in range(2)]
            P.op("pool", lambda e: e.memset(xt[0][:], 0.0), writes=[B_xt[0]])
            npt = 0
            for t in [16] + list(range(16)):
                i = 0 if t == 16 else 1
                g = 0 if t < 16 else 1
                if t < 16:
                    P.dma("sp", lambda e, t=t, i=i: e.dma_start(out=xt[i][:], in_=x_p[128 * t:128 * (t + 1), :]),
                          B_xt[i], writes=[B_xt[i]])
                else:
                    P.dma("sp", lambda e, i=i: e.dma_start(out=xt[i][0:SD, :], in_=x_s), B_xt[i], writes=[B_xt[i]])
                P.op("act", lambda e, i=i: e.activation(out=junk[:], in_=xt[i][:], func=AF.Square,
                                                        accum_out=st[i][:, 0:1]),
                     reads=[B_xt[i]], writes=[B_junk, B_st[i]])
                P.op("dve", lambda e, i=i: e.tensor_scalar(out=st[i][:, 1:2], in0=st[i][:, 0:1], scalar1=1.0 / D,
                                                           scalar2=EPS, op0=ALU.mult, op1=ALU.add),
                     reads=[B_st[i]], writes=[B_st[i]])
                P.op("act", lambda e, i=i: e.activation(out=st[i][:, 2:3], in_=st[i][:, 1:2], func=AF.Sqrt),
                     reads=[B_st[i]], writes=[B_st[i]])
                P.op("dve", lambda e, i=i: e.reciprocal(out=st[i][:, 3:4], in_=st[i][:, 2:3]),
                     reads=[B_st[i]], writes=[B_st[i]])
                P.op("dve", lambda e, i=i, g=g: e.scalar_tensor_tensor(
                    out=tmp[i][:], in0=xt[i][:], scalar=st[i][:, 3:4], in1=bc[:, g, 0, :], op0=ALU.mult, op1=ALU.mult),
                    reads=[B_xt[i], B_st[i], B_bc], writes=[B_tmp[i]])
                P.op("pool", lambda e, i=i, g=g: e.tensor_tensor(out=hb[i][:], in0=tmp[i][:], in1=bc[:, g, 1, :],
                                                                 op=ALU.add),
                     reads=[B_tmp[i], B_bc], writes=[B_hb[i]])
                for q4 in range(4):
                    pi = npt % 2; npt += 1
                    P.op("pe", [lambda e, i=i, c=c, pi=pi: e.transpose(out=pt[pi][:, c % 4, :],
                                                                       in_=hb[i][:, 128 * c:128 * (c + 1)],
                                                                       identity=ident_b[:])
                                for c in range(4 * q4, 4 * q4 + 4)],
                         reads=[B_hb[i], B_ident], writes=[B_pt[pi]])
                    P.op("act", lambda e, t=t, q4=q4, pi=pi: e.copy(
                        out=hT[:, 4 * q4:4 * q4 + 4, 128 * t:128 * (t + 1)], in_=pt[pi][:]),
                        reads=[B_pt[pi]], writes=[B_hT[t]])
            P.end()
        if stage <= 1:
            return nc

        P.begin()
        with ExitStack() as ph:
            def sbp(name, shape, dt):
                return ph.enter_context(nc.sbuf_tensor(name, list(shape), dt))

            def psp(name, shape, dt):
                return ph.enter_context(nc.psum_tensor(name, list(shape), dt))
            NW = 3
            wslots = [sbp(f"w2_{i}", [128, KC, 512], BF16) for i in range(NW)]
            B_w = [Buf(f"w2_{i}") for i in range(NW)]
            pj = [psp(f"pj{i}", [128, 512], F32) for i in range(3)]
            B_pj = [Buf(f"pj{i}") for i in range(3)]
            pt = [psp(f"ptq{i}", [128, 4, 128], BF16) for i in range(2)]
            B_pt = [Buf(f"ptq{i}") for i in range(2)]
            cs = sbp("cs", [128, NT, 128], F32); B_cs = Buf("cs")
            P.dma("sp", lambda e: e.dma_start(out=cs[:], in_=rope_cs.rearrange("(t p) n -> p t n", p=128)),
                  B_cs, writes=[B_cs])
            NS = 3
            f32s = [sbp(f"f32s{i}", [128, 512], F32) for i in range(NS)]
            B_f32s = [Buf(f"f32s{i}") for i in range(NS)]
            ra = [sbp(f"ra{i}", [128, 512], F32) for i in range(2)]
            B_ra = [Buf(f"ra{i}") for i in range(2)]
            b16s = [sbp(f"b16s{i}", [128, 512], BF16) for i in range(NS)]
            B_b16s = [Buf(f"b16s{i}") for i in range(NS)]
            tT = [sbp(f"tT{i}", [128, 4, 128], BF16) for i in range(NS)]
            B_tT = [Buf(f"tT{i}") for i in range(NS)]
            it = 0
            pendT = []
            outs_p = {2: o_sbk_p, 3: o_sbk_p, 4: o_sbv_p, 5: o_sbv_p, 8: o_dfk_p, 9: o_dfk_p, 10: o_dfv_p, 11: o_dfv_p}
            outs_s = {2: o_sbk_s, 3: o_sbk_s, 4: o_sbv_s, 5: o_sbv_s, 8: o_dfk_s, 9: o_dfk_s, 10: o_dfv_s, 11: o_dfv_s}
            import os
            _js = [int(v) for v in os.environ.get('PH2_JS', ','.join(str(v) for v in range(20))).split(',')]
            for j in _js:
                s = j % NW
                P.dma("pool", lambda e, j=j, s=s: e.dma_start(
                    out=wslots[s][:], in_=w_in[:, 512 * j:512 * (j + 1)].rearrange("(c p) n -> p c n", p=128)),
                    B_w[s], writes=[B_w[s]])
                for t in range(NT):
                    k = it % 3; k2 = it % 2; ks = it % NS; it += 1
                    nr = tok_rows(t)
                    pp = pj[k]; bp = B_pj[k]
                    P.op("pe", [lambda e, c=c, s=s, pp=pp, t=t: e.matmul(
                        out=pp[:], lhsT=hT[:, c, 128 * t:128 * (t + 1)], rhs=wslots[s][:, c, :],
                        start=(c == 0), stop=(c == KC - 1)) for c in range(KC)],
                        reads=[B_hT[t], B_w[s]], writes=[bp])
                    if pendT:
                        pendT.pop(0)()
                    half = j % 2
                    grp = j // 2
                    if grp >= 6:
                        gc = (j - 12) * 512
                        P.op("act", lambda e, pp=pp, ks=ks: e.activation(out=b16s[ks][:], in_=pp[:], func=AF.Sigmoid),
                             reads=[bp], writes=[B_b16s[ks]])
                        P.dma("sp", lambda e, ks=ks, t=t, gc=gc: e.dma_start(
                            out=gates_d[128 * t:128 * (t + 1), gc:gc + 512], in_=b16s[ks][:]),
                            B_b16s[ks], reads=[B_b16s[ks]])
                        continue
                    rope = grp in (3, 4)
                    need_T = grp in (0, 1, 3, 4)
                    need_out = grp in (1, 2, 4, 5)
                    src32 = None
                    if rope:
                        xa = pp[:].rearrange("p (h two d) -> p h two d", h=4, two=2)
                        cosb = cs[:, t, 0:64]
                        sinb = cs[:, t, 64:128]
                        A = ra[k2]; Bf = f32s[ks]
                        Av = A[:].rearrange("p (h two d) -> p h two d", h=4, two=2)
                        Bv = Bf[:].rearrange("p (h two d) -> p h two d", h=4, two=2)

                        cos4 = cosb.unsqueeze(1).unsqueeze(1).to_broadcast([128, 4, 2, 64])
                        sin4 = sinb.unsqueeze(1).to_broadcast([128, 4, 64])
                        P.op("dve", lambda e, xa=xa, Av=Av, cos4=cos4: e.tensor_tensor(
                            out=Av, in0=xa, in1=cos4, op=ALU.mult), reads=[bp, B_cs], writes=[B_ra[k2]])
                        P.op("dve", [lambda e, xa=xa, Bv=Bv, sin4=sin4: e.tensor_tensor(
                            out=Bv[:, :, 0, :], in0=xa[:, :, 1, :], in1=sin4, op=ALU.mult),
                            lambda e, xa=xa, Bv=Bv, sin4=sin4: e.tensor_tensor(
                            out=Bv[:, :, 1, :], in0=xa[:, :, 0, :], in1=sin4, op=ALU.mult)],
                            reads=[bp, B_cs], writes=[B_f32s[ks]])
                        P.op("pool", [lambda e, Av=Av, Bv=Bv: e.tensor_tensor(out=Bv[:, :, 0, :], in0=Av[:, :, 0, :],
                                                                              in1=Bv[:, :, 0, :], op=ALU.subtract),
                                      lambda e, Av=Av, Bv=Bv: e.tensor_tensor(out=Bv[:, :, 1, :], in0=Av[:, :, 1, :],
                                                                              in1=Bv[:, :, 1, :], op=ALU.add)],
                             reads=[B_ra[k2], B_f32s[ks]], writes=[B_f32s[ks]])
                        src32 = f32s[ks]
                        P.op("act", lambda e, ks=ks: e.copy(out=b16s[ks][:], in_=f32s[ks][:]),
                             reads=[B_f32s[ks]], writes=[B_b16s[ks]])
                    else:
                        if need_out:
                            P.op("act", lambda e, pp=pp, ks=ks: e.copy(out=f32s[ks][:], in_=pp[:]),
                                 reads=[bp], writes=[B_f32s[ks]])
                            src32 = f32s[ks]
                        if need_out and os.environ.get('PH2_V', 'ser') == 'ser':
                            P.op("dve", lambda e, ks=ks: e.tensor_copy(out=b16s[ks][:], in_=f32s[ks][:]),
                                 reads=[B_f32s[ks]], writes=[B_b16s[ks]])
                        else:
                            P.op("dve", lambda e, pp=pp, ks=ks: e.tensor_copy(out=b16s[ks][:], in_=pp[:]),
                                 reads=[bp], writes=[B_b16s[ks]])
                    if need_out and os.environ.get('PH2_V', '') != 'nodma':
                        cc = half * 512
                        if t < 16:
                            dst = outs_p[j][128 * t:128 * (t + 1), cc:cc + 512]
                            P.dma("sp", lambda e, dst=dst, ks=ks: e.dma_start(out=dst, in_=f32s[ks][:]),
                                  B_f32s[ks], reads=[B_f32s[ks]])
                        else:
                            dst = outs_s[j][:, cc:cc + 512]
                            P.dma("sp", lambda e, dst=dst, ks=ks: e.dma_start(out=dst, in_=f32s[ks][0:SD, :]),
                                  B_f32s[ks], reads=[B_f32s[ks]])
                    if need_T:
                        gi = {0: 0, 1: 1, 3: 2, 4: 3}[grp]
                        pi = it % 2

                        def do_T(ks=ks, pi=pi, gi=gi, half=half, t=t):
                            P.op("pe", [lambda e, ks=ks, c=c, pi=pi: e.transpose(
                                out=pt[pi][:, c, :], in_=b16s[ks][:, 128 * c:128 * (c + 1)], identity=ident_b[:])
                                for c in range(4)], reads=[B_b16s[ks], B_ident], writes=[B_pt[pi]])
                            P.op("act", lambda e, ks=ks, pi=pi: e.copy(out=tT[ks][:], in_=pt[pi][:]),
                                 reads=[B_pt[pi]], writes=[B_tT[ks]])
                            P.dma("sp", lambda e, gi=gi, half=half, t=t, ks=ks: e.dma_start(
                                out=qkT_d[gi, 4 * half:4 * half + 4, :, 128 * t:128 * (t + 1)].rearrange("h d n -> d h n"),
                                in_=tT[ks][:]), B_tT[ks], reads=[B_tT[ks]])
                        pendT.append(do_T)
                    else:
                        vi = 0 if grp == 2 else 1
                        P.dma("sp", lambda e, vi=vi, half=half, t=t, ks=ks: e.dma_start(
                            out=v_d[vi, 128 * t:128 * (t + 1), 512 * half:512 * half + 512], in_=b16s[ks][:]),
                            B_b16s[ks], reads=[B_b16s[ks]])
            while pendT:
                pendT.pop(0)()
            P.end()

    if stage <= 2:
        return nc

    kcdfT_d = dscr("kcdfT_d", [8, 128, S], BF16)
    outer2 = ExitStack()
    sboT = outer2.enter_context(nc.sbuf_tensor("sboT", [128, 8, TOK], BF16))
    dfoT = outer2.enter_context(nc.sbuf_tensor("dfoT", [128, 8, TOK], BF16))
    B_sboT = Buf("sboT"); B_dfoT = Buf("dfoT")

    P.begin()
    with ExitStack() as ph:
        def sbp(name, shape, dt):
            return ph.enter_context(nc.sbuf_tensor(name, list(shape), dt))

        def psp(name, shape, dt):
            return ph.enter_context(nc.psum_tensor(name, list(shape), dt))
        P.op("pool", lambda e: e.memset(sboT[:, :, 2048:TOK], 0.0), writes=[B_sboT])
        P.op("pool", lambda e: e.memset(dfoT[:, :, 2048:TOK], 0.0), writes=[B_dfoT])
        Tm = sbp("Tm", [128, 128], BF16); Tc = sbp("Tc", [128, 128], BF16)
        msk = sbp("msk", [128, 4, 512], BF16)
        B_T = Buf("T"); B_msk = Buf("msk")
        P.op("pool", lambda e: e.memset(Tm[:], 1.0), writes=[B_T])
        P.op("pool", lambda e: e.affine_select(out=Tm[:], in_=Tm[:], pattern=[[-1, 128]], compare_op=ALU.is_ge,
                                               fill=0.0, base=0, channel_multiplier=1), reads=[B_T], writes=[B_T])
        P.op("pool", lambda e: e.memset(Tc[:], 1.0), reads=[B_T], writes=[B_T])
        P.op("pool", lambda e: e.affine_select(out=Tc[:], in_=Tc[:], pattern=[[1, 128]], compare_op=ALU.is_ge,
                                               fill=0.0, base=-1, channel_multiplier=-1), reads=[B_T], writes=[B_T])
        P.op("pool", lambda e: e.memset(msk[:], 1.0), writes=[B_msk])
        for d in range(4):
            P.op("pool", lambda e, d=d: e.affine_select(out=msk[:, d, :], in_=msk[:, d, :], pattern=[[1, 512]],
                                                        compare_op=ALU.is_ge, fill=0.0, base=-128 * d - 1,
                                                        channel_multiplier=-1), reads=[B_msk], writes=[B_msk])
        qT = [sbp(f"qT{i}", [128, TOK], BF16) for i in range(2)]
        kT = [sbp(f"kT{i}", [128, TOK], BF16) for i in range(2)]
        vv = [sbp(f"vv{i}", [128, NT, 128], BF16) for i in range(2)]
        kcb = [sbp(f"kcb{i}", [128, 16, 128], BF16) for i in range(2)]
        vcb = [sbp(f"vcb{i}", [128, 16, 128], BF16) for i in range(2)]
        kcT = [sbp(f"kcT{i}", [128, S], BF16) for i in range(2)]
        kdb = [sbp(f"kdb{i}", [128, 16, 128], BF16) for i in range(2)]
        kdT = [sbp(f"kdT{i}", [128, S], BF16) for i in range(2)]
        B_q = [Buf(f"q{i}") for i in range(2)]; B_k = [Buf(f"k{i}") for i in range(2)]
        B_v = [Buf(f"v{i}") for i in range(2)]; B_kcb = [Buf(f"kcb{i}") for i in range(2)]
        B_vcb = [Buf(f"vcb{i}") for i in range(2)]; B_kcT = [Buf(f"kcT{i}") for i in range(2)]
        B_kdb = [Buf(f"kdb{i}") for i in range(2)]; B_kdT = [Buf(f"kdT{i}") for i in range(2)]
        NWK = 5
        e32 = [sbp(f"e32_{i}", [128, 512], F32) for i in range(NWK)]
        Lall = [sbp(f"Lall_{i}", [128, NT, 512], BF16) for i in range(2)]
        B_Lall = [[Buf(f"Lall{i}_{j}") for j in range(NT)] for i in range(2)]
        ones3 = sbp("ones3", [128, 128], BF16)
        P.op("pool", lambda e: e.memset(ones3[:], 1.0), reads=[B_T], writes=[B_T])
        g32 = [sbp(f"g32_{i}", [128, 512], F32) for i in range(NWK)]
        ab = [sbp(f"ab_{i}", [128, 512], BF16) for i in range(NWK)]
        B_e = [Buf(f"e{i}") for i in range(NWK)]
        B_g = [Buf(f"g{i}") for i in range(NWK)]; B_ab = [Buf(f"ab{i}") for i in range(NWK)]
        zps = [psp(f"zps{i}", [128, 512], F32) for i in range(2)]
        B_z = [Buf(f"z{i}") for i in range(2)]
        Cps = [psp(f"Cps{i}", [128, 512], F32) for i in range(2)]; B_C = [Buf(f"C{i}", True) for i in range(2)]
        Ops = [psp(f"Ops{i}", [128, 512], F32) for i in range(2)]; B_O = [Buf(f"O{i}", True) for i in range(2)]
        ptk = [psp(f"ptk{i}", [128, 4, 128], BF16) for i in range(2)]
        B_ptk = [Buf(f"ptk{i}") for i in range(2)]
        cnt = {"w": 0, "z": 0, "p": 0, "s": 0, "c": 0, "o": 0}

        def cache_T(src_b, B_src, dstT, B_dst):
            for q4 in range(4):
                pi = cnt["p"] % 2; cnt["p"] += 1
                P.op("pe", [lambda e, c=c, pi=pi: e.transpose(out=ptk[pi][:, c % 4, :], in_=src_b[:, c, :],
                                                             identity=ident_b[:]) for c in range(4 * q4, 4 * q4 + 4)],
                     reads=[B_src, B_ident], writes=[B_ptk[pi]])
                P.op("dve", lambda e, q4=q4, pi=pi: e.tensor_copy(
                    out=dstT[:, 512 * q4:512 * (q4 + 1)].rearrange("p (c n) -> p c n", c=4), in_=ptk[pi][:]),
                    reads=[B_ptk[pi]], writes=[B_dst])

        def sb_sweep(h, hb, q0, N, keytiles):
            nk = len(keytiles)
            sw = cnt["s"] % 2; cnt["s"] += 1
            oi = cnt["o"] % 2; cnt["o"] += 1

            def stage_a(idx):
                kt_ap, v_ap, m_ap, rk, rv = keytiles[idx]
                w = cnt["w"] % NWK; cnt["w"] += 1
                zi = cnt["z"] % 2; cnt["z"] += 1
                P.op("pe", lambda e, zi=zi, kt_ap=kt_ap: e.matmul(out=zps[zi][:, 0:N], lhsT=kt_ap,
                                                                  rhs=qT[hb][:, q0:q0 + N], start=True, stop=True),
                     reads=[B_q[hb]] + rk, writes=[B_z[zi]])
                P.op("act", lambda e, zi=zi, w=w: e.activation(out=e32[w][:, 0:N], in_=zps[zi][:, 0:N], func=AF.Exp,
                                                               scale=SCALE), reads=[B_z[zi]], writes=[B_e[w]])
                if m_ap is not None:
                    P.op("pool", lambda e, w=w, m_ap=m_ap: e.tensor_tensor(out=e32[w][:, 0:N], in0=e32[w][:, 0:N],
                                                                           in1=m_ap, op=ALU.mult),
                         reads=[B_e[w], B_msk], writes=[B_e[w]])
                P.op("act", lambda e, w=w, idx=idx: e.activation(out=Lall[sw][:, idx, 0:N], in_=e32[w][:, 0:N],
                                                                 func=AF.Ln, bias=1.0),
                     reads=[B_e[w]], writes=[B_Lall[sw][idx]])
                return w

            def stage_b(idx, w):
                ci = cnt["c"] % 2; cnt["c"] += 1
                fl = [lambda e, b=b, ci=ci: e.matmul(out=Cps[ci][:, 0:N], lhsT=ones3[:], rhs=Lall[sw][:, b, 0:N],
                                                     start=(b == 0), stop=False) for b in range(idx)]
                fl.append(lambda e, idx=idx, ci=ci: e.matmul(out=Cps[ci][:, 0:N], lhsT=Tm[:], rhs=Lall[sw][:, idx, 0:N],
                                                             start=(idx == 0), stop=True))
                P.op("pe", fl, reads=[B_Lall[sw][b] for b in range(idx + 1)] + [B_T], writes=[B_C[ci]])
                P.op("act", lambda e, w=w, ci=ci: e.activation(out=g32[w][:, 0:N], in_=Cps[ci][:, 0:N], func=AF.Exp, scale=-1.0),
                     reads=[B_C[ci]], writes=[B_g[w]])
                P.op("dve", lambda e, w=w: e.tensor_tensor(out=ab[w][:, 0:N], in0=e32[w][:, 0:N], in1=g32[w][:, 0:N],
                                                           op=ALU.mult), reads=[B_e[w], B_g[w]], writes=[B_ab[w]])

            def stage_c(idx, w):
                kt_ap, v_ap, m_ap, rk, rv = keytiles[idx]
                first = idx == 0; last = idx == nk - 1
                P.op("pe", lambda e, w=w, v_ap=v_ap, first=first, last=last: e.matmul(
                    out=Ops[oi][:, 0:N], lhsT=v_ap, rhs=ab[w][:, 0:N], start=first, stop=last),
                    reads=[B_ab[w]] + rv, writes=[B_O[oi]])

            ws_ = {}
            for it_ in range(nk + 2):
                if it_ < nk:
                    ws_[it_] = stage_a(it_)
                if 0 <= it_ - 1 < nk:
                    stage_b(it_ - 1, ws_[it_ - 1])
                if 0 <= it_ - 2 < nk:
                    stage_c(it_ - 2, ws_[it_ - 2])
            P.op("dve", lambda e: e.tensor_copy(out=sboT[:, h, q0:q0 + N], in_=Ops[oi][:, 0:N]),
                 reads=[B_O[oi]], writes=[B_sboT])

        for h in range(8):
            hb = h % 2
            P.dma("sp", lambda e, h=h, hb=hb: e.dma_start(out=qT[hb][:], in_=qkT_d[0, h, :, :]), B_q[hb], writes=[B_q[hb]])
            P.dma("sp", lambda e, h=h, hb=hb: e.dma_start(out=kT[hb][:], in_=qkT_d[1, h, :, :]), B_k[hb], writes=[B_k[hb]])
            P.dma("sp", lambda e, h=h, hb=hb: e.dma_start(
                out=vv[hb][:], in_=v_d[0, :, 128 * h:128 * (h + 1)].rearrange("(t p) d -> p t d", p=128)),
                B_v[hb], writes=[B_v[hb]])
            P.dma("pool", lambda e, h=h, hb=hb: e.dma_start(
                out=kcb[hb][:], in_=c_sb_k[:, 128 * h:128 * (h + 1)].rearrange("(t p) d -> p t d", p=128)),
                B_kcb[hb], writes=[B_kcb[hb]])
            P.dma("pool", lambda e, h=h, hb=hb: e.dma_start(
                out=vcb[hb][:], in_=c_sb_v[:, 128 * h:128 * (h + 1)].rearrange("(t p) d -> p t d", p=128)),
                B_vcb[hb], writes=[B_vcb[hb]])
            P.dma("pool", lambda e, h=h, hb=hb: e.dma_start(
                out=kdb[hb][:], in_=c_df_k[:, 128 * h:128 * (h + 1)].rearrange("(t p) d -> p t d", p=128)),
                B_kdb[hb], writes=[B_kdb[hb]])
            cache_T(kcb[hb], B_kcb[hb], kcT[hb], B_kcT[hb])
            cache_T(kdb[hb], B_kdb[hb], kdT[hb], B_kdT[hb])
            P.dma("sp", lambda e, h=h, hb=hb: e.dma_start(out=kcdfT_d[h, :, :], in_=kdT[hb][:]), B_kdT[hb],
                  reads=[B_kdT[hb]])
            for i in range(4):
                kts = []
                for j in range(4 * i + 3, -1, -1):
                    m_ap = msk[:, j - 4 * i, :] if j >= 4 * i else None
                    kts.append((kT[hb][:, 128 * j:128 * (j + 1)], vv[hb][:, j, :], m_ap, [B_k[hb]], [B_v[hb]]))
                sb_sweep(h, hb, 512 * i, 512, kts)
            kts = [(kT[hb][:, 2048:2176], vv[hb][:, 16, :], msk[:, 0, 0:SD], [B_k[hb]], [B_v[hb]])]
            for j in range(15, -1, -1):
                kts.append((kcT[hb][:, 128 * j:128 * (j + 1)], vcb[hb][:, j, :], None, [B_kcT[hb]], [B_vcb[hb]]))
            sb_sweep(h, hb, 2048, SD, kts)
        P.end()
    if stage <= 3:
        pass
        return nc

    P.begin()
    with ExitStack() as ph:
        def sbp(name, shape, dt):
            return ph.enter_context(nc.sbuf_tensor(name, list(shape), dt))

        def psp(name, shape, dt):
            return ph.enter_context(nc.psum_tensor(name, list(shape), dt))
        ones_b = sbp("ones_b", [128, 128], BF16); ones_f = sbp("ones_f", [128, 128], F32)
        mskd = sbp("mskd", [128, 4, 512], BF16); mskv = sbp("mskv", [128, SD], BF16)
        B_cst = Buf("cst4")
        P.op("pool", lambda e: e.memset(ones_b[:], 1.0), writes=[B_cst])
        P.op("pool", lambda e: e.memset(ones_f[:], 1.0), writes=[B_cst])
        P.op("pool", lambda e: e.memset(mskd[:], 1.0), writes=[B_cst])
        for d in range(4):
            P.op("pool", lambda e, d=d: e.affine_select(
                out=mskd[:, d, :].rearrange("p (a b) -> p a b", a=8), in_=mskd[:, d, :].rearrange("p (a b) -> p a b", a=8),
                pattern=[[64, 8], [0, 64]], compare_op=ALU.is_ge, fill=0.0, base=63 - 128 * d, channel_multiplier=-1),
                reads=[B_cst], writes=[B_cst])
        P.op("pool", lambda e: e.memset(mskv[:], 1.0), reads=[B_cst], writes=[B_cst])
        P.op("pool", lambda e: e.affine_select(out=mskv[:], in_=mskv[:], pattern=[[0, SD]], compare_op=ALU.is_ge,
                                               fill=0.0, base=SD - 1, channel_multiplier=-1), reads=[B_cst], writes=[B_cst])
        lt = sbp("lt", [128, 4, 128], F32); lw = sbp("lw", [128, 2, 128], F32); ls = sbp("ls", [128, 8], F32)
        gsc = sbp("gsc", [128, 2], F32)
        B_lt = Buf("lt"); B_ls = Buf("ls"); B_gsc = Buf("gsc")
        for r in range(4):
            P.dma("sp", lambda e, r=r: e.dma_start(out=lt[:, r, :], in_=lamv[r, :].partition_broadcast(128)), B_lt, writes=[B_lt])
        for half in range(2):
            P.dma("sp", lambda e, half=half: e.dma_start(out=gsc[:, half:half + 1], in_=g_subln[128 * half:128 * (half + 1), :]),
                  B_gsc, writes=[B_gsc])
        P.op("dve", lambda e: e.tensor_scalar(out=gsc[:], in0=gsc[:], scalar1=1.0 - LAM_INIT, scalar2=None, op0=ALU.mult),
             reads=[B_gsc], writes=[B_gsc])
        B_lw = Buf("lw")
        P.op("dve", lambda e: e.tensor_tensor(out=lw[:, 0, :], in0=lt[:, 0, :], in1=lt[:, 1, :], op=ALU.mult), reads=[B_lt], writes=[B_lw])
        P.op("dve", lambda e: e.tensor_tensor(out=lw[:, 1, :], in0=lt[:, 2, :], in1=lt[:, 3, :], op=ALU.mult), reads=[B_lw, B_lt], writes=[B_lw])
        P.op("dve", lambda e: e.reduce_sum(out=ls[:, 0:2], in_=lw[:], axis=AX.X), reads=[B_lw], writes=[B_ls])
        P.op("act", lambda e: e.activation(out=ls[:, 2:4], in_=ls[:, 0:2], func=AF.Exp), reads=[B_ls], writes=[B_ls])
        P.op("dve", lambda e: e.tensor_tensor(out=ls[:, 4:5], in0=ls[:, 3:4], in1=ls[:, 2:3], op=ALU.subtract), reads=[B_ls], writes=[B_ls])
        P.op("dve", lambda e: e.tensor_scalar(out=ls[:, 5:6], in0=ls[:, 4:5], scalar1=-LAM_INIT, scalar2=None, op0=ALU.add),
             reads=[B_ls], writes=[B_ls])
        neglam = ls[:, 5:6]

        qT2 = [[sbp(f"dq{i}{c}", [128, TOK], BF16) for c in range(2)] for i in range(2)]
        kT2 = [[sbp(f"dk{i}{c}", [128, TOK], BF16) for c in range(2)] for i in range(2)]
        kc2 = [[sbp(f"dkc{i}{c}", [128, S], BF16) for c in range(2)] for i in range(2)]
        vv2 = [sbp(f"dv{i}", [128, NT, 256], BF16) for i in range(2)]
        vc2 = [sbp(f"dvc{i}", [128, 16, 256], BF16) for i in range(2)]
        B_q2 = [Buf(f"dq{i}") for i in range(2)]; B_k2 = [Buf(f"dk{i}") for i in range(2)]
        B_kc2 = [Buf(f"dkc{i}") for i in range(2)]; B_v2 = [Buf(f"dv{i}") for i in range(2)]
        B_vc2 = [Buf(f"dvc{i}") for i in range(2)]
        NWK = 4
        pb = [sbp(f"pb{i}", [128, 512], BF16) for i in range(NWK)]
        B_pb = [Buf(f"pb{i}") for i in range(NWK)]
        zps = [psp(f"dz{i}", [128, 512], F32) for i in range(2)]
        B_z = [Buf(f"dz{i}") for i in range(2)]
        pv = [[psp(f"pv{c}{hf}", [128, 512], F32) for hf in range(2)] for c in range(2)]
        den = [psp(f"den{c}", [128, 512], F32) for c in range(2)]
        B_acc = [Buf(f"acc{c}") for c in range(2)]
        rden = [sbp(f"rden{c}", [128, 512], F32) for c in range(2)]
        B_rden = [Buf(f"rden{c}") for c in range(2)]
        oo = [[sbp(f"oo{c}{hf}", [128, 512], F32) for hf in range(2)] for c in range(2)]
        B_oo = [Buf(f"oo{c}") for c in range(2)]
        at = [sbp(f"at{hf}", [128, 512], F32) for hf in range(2)]
        sq = [sbp(f"sq{hf}", [128, 512], F32) for hf in range(2)]
        rs = sbp("rs", [128, 512], F32); rs2 = sbp("rs2", [128, 512], F32)
        B_at = Buf("at"); B_sq = Buf("sq"); B_rs = Buf("rs"); B_rs2 = Buf("rs2")
        cnt = {"w": 0, "z": 0}

        def df_sweep(h, hb, q0, N, keytiles):
            nk = len(keytiles)
            steps = [(idx, c) for idx in range(nk) for c in range(2)]
            pend = []

            def stage_a(idx, c):
                kaps, vap, m_ap, rk, rv = keytiles[idx]
                w = cnt["w"] % NWK; cnt["w"] += 1
                zi = cnt["z"] % 2; cnt["z"] += 1
                P.op("pe", lambda e, zi=zi, c=c, kaps=kaps: e.matmul(
                    out=zps[zi][:, 0:N], lhsT=kaps[c], rhs=qT2[hb][c][:, q0:q0 + N], start=True, stop=True),
                    reads=[B_q2[hb]] + rk, writes=[B_z[zi]])
                P.op("act", lambda e, zi=zi, w=w: e.activation(out=pb[w][:, 0:N], in_=zps[zi][:, 0:N], func=AF.Exp,
                                                               scale=SCALE), reads=[B_z[zi]], writes=[B_pb[w]])
                if m_ap is not None:
                    P.op("pool", lambda e, w=w, m_ap=m_ap: e.tensor_tensor(
                        out=pb[w][:, 0:N], in0=pb[w][:, 0:N], in1=m_ap, op=ALU.mult),
                        reads=[B_pb[w], B_cst], writes=[B_pb[w]])
                return w

            def stage_b(idx, c, w):
                kaps, vap, m_ap, rk, rv = keytiles[idx]
                first = idx == 0; last = idx == nk - 1
                P.op("pe", [lambda e, w=w, c=c, vap=vap, first=first, last=last: e.matmul(
                                out=pv[c][0][:, 0:N], lhsT=vap[:, 0:128], rhs=pb[w][:, 0:N], start=first, stop=last),
                            lambda e, w=w, c=c, vap=vap, first=first, last=last: e.matmul(
                                out=pv[c][1][:, 0:N], lhsT=vap[:, 128:256], rhs=pb[w][:, 0:N], start=first, stop=last),
                            lambda e, w=w, c=c, first=first, last=last: e.matmul(
                                out=den[c][:, 0:N], lhsT=ones_b[:], rhs=pb[w][:, 0:N], start=first, stop=last)],
                     reads=[B_pb[w], B_cst] + rv, writes=[B_acc[c]])

            for (idx, c) in steps:
                w = stage_a(idx, c)
                pend.append((idx, c, w))
                if len(pend) > 1:
                    stage_b(*pend.pop(0))
            while pend:
                stage_b(*pend.pop(0))
            for c in range(2):
                P.op("dve", lambda e, c=c: e.reciprocal(out=rden[c][:, 0:N], in_=den[c][:, 0:N]),
                     reads=[B_acc[c]], writes=[B_rden[c]])
                P.op("dve", [lambda e, c=c, hf=hf: e.tensor_tensor(out=oo[c][hf][:, 0:N], in0=pv[c][hf][:, 0:N],
                                                                  in1=rden[c][:, 0:N], op=ALU.mult) for hf in range(2)],
                     reads=[B_acc[c], B_rden[c]], writes=[B_oo[c]])
            P.op("dve", [lambda e, hf=hf: e.scalar_tensor_tensor(out=at[hf][:, 0:N], in0=oo[1][hf][:, 0:N], scalar=neglam,
                                                                 in1=oo[0][hf][:, 0:N], op0=ALU.mult, op1=ALU.add)
                         for hf in range(2)], reads=[B_oo[0], B_oo[1], B_ls], writes=[B_at])
            P.op("act", lambda e: e.activation(out=sq[0][:, 0:N], in_=at[0][:, 0:N], func=AF.Square), reads=[B_at], writes=[B_sq])
            P.op("act", lambda e: e.activation(out=sq[1][:, 0:N], in_=at[1][:, 0:N], func=AF.Square), reads=[B_at, B_sq], writes=[B_sq])
            zi = cnt["z"] % 2; cnt["z"] += 1
            P.op("pe", [lambda e, zi=zi, hf=hf: e.matmul(out=zps[zi][:, 0:N], lhsT=ones_f[:], rhs=sq[hf][:, 0:N],
                                                        start=(hf == 0), stop=(hf == 1)) for hf in range(2)],
                 reads=[B_sq, B_cst], writes=[B_z[zi]])
            P.op("dve", lambda e, zi=zi: e.tensor_scalar(out=rs2[:, 0:N], in0=zps[zi][:, 0:N], scalar1=1.0 / 256, scalar2=EPS,
                                                         op0=ALU.mult, op1=ALU.add), reads=[B_z[zi]], writes=[B_rs2])
            P.op("act", lambda e: e.activation(out=rs[:, 0:N], in_=rs2[:, 0:N], func=AF.Sqrt), reads=[B_rs2], writes=[B_rs])
            P.op("dve", lambda e: e.reciprocal(out=rs2[:, 0:N], in_=rs[:, 0:N]), reads=[B_rs], writes=[B_rs2])
            for hf in range(2):
                P.op("dve", lambda e, hf=hf: e.scalar_tensor_tensor(
                    out=dfoT[:, 2 * h + hf, q0:q0 + N], in0=at[hf][:, 0:N], scalar=gsc[:, hf:hf + 1], in1=rs2[:, 0:N],
                    op0=ALU.mult, op1=ALU.mult), reads=[B_at, B_gsc, B_rs2], writes=[B_dfoT])

        for h in range(4):
            hb = h % 2
            for c in range(2):
                P.dma("sp", lambda e, h=h, hb=hb, c=c: e.dma_start(out=qT2[hb][c][:], in_=qkT_d[2, 2 * h + c, :, :]),
                      B_q2[hb], writes=[B_q2[hb]])
                P.dma("sp", lambda e, h=h, hb=hb, c=c: e.dma_start(out=kT2[hb][c][:], in_=qkT_d[3, 2 * h + c, :, :]),
                      B_k2[hb], writes=[B_k2[hb]])
                P.dma("sp", lambda e, h=h, hb=hb, c=c: e.dma_start(out=kc2[hb][c][:], in_=kcdfT_d[2 * h + c, :, :]),
                      B_kc2[hb], writes=[B_kc2[hb]])
            P.dma("sp", lambda e, h=h, hb=hb: e.dma_start(
                out=vv2[hb][:], in_=v_d[1, :, 256 * h:256 * (h + 1)].rearrange("(t p) d -> p t d", p=128)),
                B_v2[hb], writes=[B_v2[hb]])
            P.dma("pool", lambda e, h=h, hb=hb: e.dma_start(
                out=vc2[hb][:], in_=c_df_v[:, 256 * h:256 * (h + 1)].rearrange("(t p) d -> p t d", p=128)),
                B_vc2[hb], writes=[B_vc2[hb]])
            for i in range(4):
                kts = []
                for j in range(4 * i + 4):
                    m_ap = mskd[:, j - 4 * i, :] if j >= 4 * i else None
                    kts.append(([kT2[hb][c][:, 128 * j:128 * (j + 1)] for c in range(2)], vv2[hb][:, j, :], m_ap,
                                [B_k2[hb]], [B_v2[hb]]))
                df_sweep(h, hb, 512 * i, 512, kts)
            kts = [([kT2[hb][c][:, 2048:2176] for c in range(2)], vv2[hb][:, 16, :], mskv[:], [B_k2[hb]], [B_v2[hb]])]
            for j in range(16):
                kts.append(([kc2[hb][c][:, 128 * j:128 * (j + 1)] for c in range(2)], vc2[hb][:, j, :], None,
                            [B_kc2[hb]], [B_vc2[hb]]))
            df_sweep(h, hb, 2048, SD, kts)
        P.end()
    if stage <= 4:
        pass
        return nc

    mT_d = dscr("mT_d", [KC, 128, TOK], BF16)
    P.begin()
    with ExitStack() as ph:
        def sbp(name, shape, dt):
            return ph.enter_context(nc.sbuf_tensor(name, list(shape), dt))

        def psp(name, shape, dt):
            return ph.enter_context(nc.psum_tensor(name, list(shape), dt))
        wa = [sbp(f"wa{i}", [128, 8, 512], BF16) for i in range(2)]
        wb = [sbp(f"wb{i}", [128, 8, 512], BF16) for i in range(2)]
        B_wa = [Buf(f"wa{i}") for i in range(2)]; B_wb = [Buf(f"wb{i}") for i in range(2)]
        gsb = [sbp(f"gsb{i}", [128, 512], BF16) for i in range(2)]
        gdf = [sbp(f"gdf{i}", [128, 512], BF16) for i in range(2)]
        B_gsb = [Buf(f"gsb{i}") for i in range(2)]; B_gdf = [Buf(f"gdf{i}") for i in range(2)]
        t1 = [sbp(f"t1_{i}", [128, 512], F32) for i in range(2)]
        t2 = [sbp(f"t2_{i}", [128, 512], F32) for i in range(2)]
        mb = [sbp(f"mb_{i}", [128, 512], BF16) for i in range(2)]
        mts = [sbp(f"mts_{i}", [128, 4, 128], BF16) for i in range(2)]
        B_t1 = [Buf(f"t1{i}") for i in range(2)]; B_t2 = [Buf(f"t2{i}") for i in range(2)]
        B_mb = [Buf(f"mb{i}") for i in range(2)]; B_mts = [Buf(f"mts{i}") for i in range(2)]
        pA = [psp(f"pA{i}", [128, 512], F32) for i in range(2)]
        pB = [psp(f"pB{i}", [128, 512], F32) for i in range(2)]
        B_pA = [Buf(f"pA{i}", True) for i in range(2)]; B_pB = [Buf(f"pB{i}", True) for i in range(2)]
        ptm = [psp(f"ptm{i}", [128, 4, 128], BF16) for i in range(2)]
        B_ptm = [Buf(f"ptm{i}", True) for i in range(2)]
        it = 0
        pend5 = []
        for j in range(4):
            s2 = j % 2
            P.dma("pool", lambda e, j=j, s2=s2: e.dma_start(
                out=wa[s2][:], in_=w_pa[:, 512 * j:512 * (j + 1)].rearrange("(c p) n -> p c n", p=128)),
                B_wa[s2], writes=[B_wa[s2]])
            P.dma("pool", lambda e, j=j, s2=s2: e.dma_start(
                out=wb[s2][:], in_=w_pb[:, 512 * j:512 * (j + 1)].rearrange("(c p) n -> p c n", p=128)),
                B_wb[s2], writes=[B_wb[s2]])
            for t in range(NT):
                k = it % 2; it += 1
                P.dma("sp", lambda e, t=t, j=j, k=k: e.dma_start(
                    out=gsb[k][:], in_=gates_d[128 * t:128 * (t + 1), 512 * j:512 * (j + 1)]), B_gsb[k], writes=[B_gsb[k]])
                P.dma("sp", lambda e, t=t, j=j, k=k: e.dma_start(
                    out=gdf[k][:], in_=gates_d[128 * t:128 * (t + 1), 2048 + 512 * j:2048 + 512 * (j + 1)]),
                    B_gdf[k], writes=[B_gdf[k]])
                P.op("pe", [lambda e, c=c, k=k, t=t, s2=s2: e.matmul(
                    out=pA[k][:], lhsT=sboT[:, c, 128 * t:128 * (t + 1)], rhs=wa[s2][:, c, :], start=(c == 0), stop=(c == 7))
                    for c in range(8)], reads=[B_sboT, B_wa[s2]], writes=[B_pA[k]])
                P.op("pe", [lambda e, c=c, k=k, t=t, s2=s2: e.matmul(
                    out=pB[k][:], lhsT=dfoT[:, c, 128 * t:128 * (t + 1)], rhs=wb[s2][:, c, :], start=(c == 0), stop=(c == 7))
                    for c in range(8)], reads=[B_dfoT, B_wb[s2]], writes=[B_pB[k]])
                if pend5:
                    pend5.pop(0)()
                P.op("dve", lambda e, k=k: e.tensor_tensor(out=t1[k][:], in0=pA[k][:], in1=gsb[k][:], op=ALU.mult),
                     reads=[B_pA[k], B_gsb[k]], writes=[B_t1[k]])
                P.op("dve", lambda e, k=k: e.tensor_tensor(out=t2[k][:], in0=pB[k][:], in1=gdf[k][:], op=ALU.mult),
                     reads=[B_pB[k], B_gdf[k]], writes=[B_t2[k]])
                P.op("pool", lambda e, k=k: e.tensor_tensor(out=mb[k][:], in0=t1[k][:], in1=t2[k][:], op=ALU.add),
                     reads=[B_t1[k], B_t2[k]], writes=[B_mb[k]])
                def do_T5(k=k, j=j, t=t):
                    P.op("pe", [lambda e, c=c, k=k: e.transpose(out=ptm[k][:, c, :], in_=mb[k][:, 128 * c:128 * (c + 1)],
                                                                identity=ident_b[:]) for c in range(4)],
                         reads=[B_mb[k], B_ident], writes=[B_ptm[k]])
                    P.op("act", lambda e, k=k: e.copy(out=mts[k][:], in_=ptm[k][:]), reads=[B_ptm[k]], writes=[B_mts[k]])
                    P.dma("sp", lambda e, j=j, t=t, k=k: e.dma_start(
                        out=mT_d[4 * j:4 * j + 4, :, 128 * t:128 * (t + 1)].rearrange("c d n -> d c n"), in_=mts[k][:]),
                        B_mts[k], reads=[B_mts[k]])
                pend5.append(do_T5)
        while pend5:
            pend5.pop(0)()
        P.end()
    outer2.close()
    if stage <= 5:
        return nc

    Mk_all = sb("Mk_all", [128, NT, 2, 32], F32)
    wgt_all = sb("wgt_all", [128, NT, 2], F32)
    desti = sb("desti", [128, NT, 2], I32)
    widx = sb("widx", [128, NBLK, 2], I32)
    B_Mk = Buf("Mk"); B_wgt = Buf("wgt"); B_desti = Buf("desti"); B_widx = Buf("widx")

    P.begin()
    with ExitStack() as ph:
        def sbp(name, shape, dt):
            return ph.enter_context(nc.sbuf_tensor(name, list(shape), dt))

        def psp(name, shape, dt):
            return ph.enter_context(nc.psum_tensor(name, list(shape), dt))
        mT = sbp("mT", [128, KC, TOK], BF16); B_mT = Buf("mT")
        for q4 in range(4):
            P.dma("sp", lambda e, q4=q4: e.dma_start(out=mT[:, 4 * q4:4 * q4 + 4, :],
                                                     in_=mT_d[4 * q4:4 * q4 + 4, :, :].rearrange("c d n -> d c n")),
                  B_mT, writes=[B_mT])
        gt1 = sbp("gt1", [128, 2, D], F32); B_gt1 = Buf("gt1")
        for g in range(2):
            P.dma("sp", lambda e, g=g: e.dma_start(out=gt1[:, g, :], in_=modrows[g, 2, :].partition_broadcast(128)),
                  B_gt1, writes=[B_gt1])
        wo = [sbp(f"wo{i}", [128, KC, 512], BF16) for i in range(2)]
        B_wo = [Buf(f"wo{i}") for i in range(2)]
        xb_ = [sbp(f"xblk{i}", [128, 512], F32) for i in range(3)]
        B_xb = [Buf(f"xblk{i}") for i in range(3)]
        tq = [sbp(f"tq{i}", [128, 512], F32) for i in range(3)]
        B_tq = [Buf(f"tq{i}") for i in range(3)]
        py = [psp(f"py{i}", [128, 512], F32) for i in range(3)]
        B_py = [Buf(f"py{i}", True) for i in range(3)]
        for i in range(3):
            P.op("pool", lambda e, i=i: e.memset(xb_[i][:], 0.0), writes=[B_xb[i]])
        it = 0
        for j in range(4):
            s2 = j % 2
            P.dma("pool", lambda e, j=j, s2=s2: e.dma_start(
                out=wo[s2][:], in_=w_out[:, 512 * j:512 * (j + 1)].rearrange("(c p) n -> p c n", p=128)),
                B_wo[s2], writes=[B_wo[s2]])
            for t in range(NT):
                k = it % 3; it += 1
                g = 0 if t < 16 else 1
                if t < 16:
                    P.dma("sp", lambda e, t=t, j=j, k=k: e.dma_start(
                        out=xb_[k][:], in_=x_p[128 * t:128 * (t + 1), 512 * j:512 * (j + 1)]), B_xb[k], writes=[B_xb[k]])
                else:
                    P.dma("sp", lambda e, j=j, k=k: e.dma_start(out=xb_[k][0:SD, :], in_=x_s[:, 512 * j:512 * (j + 1)]),
                          B_xb[k], writes=[B_xb[k]])
                P.op("pe", [lambda e, c=c, k=k, t=t, s2=s2: e.matmul(
                    out=py[k][:], lhsT=mT[:, c, 128 * t:128 * (t + 1)], rhs=wo[s2][:, c, :], start=(c == 0), stop=(c == KC - 1))
                    for c in range(KC)], reads=[B_mT, B_wo[s2]], writes=[B_py[k]])
                P.op("dve", lambda e, k=k, g=g, j=j: e.tensor_tensor(out=tq[k][:], in0=py[k][:],
                                                                     in1=gt1[:, g, 512 * j:512 * (j + 1)], op=ALU.mult),
                     reads=[B_py[k], B_gt1], writes=[B_tq[k]])
                P.op("pool", lambda e, k=k: e.tensor_tensor(out=tq[k][:], in0=tq[k][:], in1=xb_[k][:], op=ALU.add),
                     reads=[B_tq[k], B_xb[k]], writes=[B_tq[k]])
                P.dma("sp", lambda e, t=t, j=j, k=k: e.dma_start(
                    out=x2_d[128 * t:128 * (t + 1), 512 * j:512 * (j + 1)], in_=tq[k][:]), B_tq[k], reads=[B_tq[k]])
        P.end()
    if stage <= 6:
        return nc

    P.begin()
    with ExitStack() as ph:
        def sbp(name, shape, dt):
            return ph.enter_context(nc.sbuf_tensor(name, list(shape), dt))

        def psp(name, shape, dt):
            return ph.enter_context(nc.psum_tensor(name, list(shape), dt))
        bc2 = sbp("bc2", [128, 2, 2, D], F32); B_bc2 = Buf("bc2")
        for g in range(2):
            for k in range(2):
                P.dma("sp", lambda e, g=g, k=k: e.dma_start(out=bc2[:, g, k, :],
                                                            in_=modrows[g, 3 + k, :].partition_broadcast(128)),
                      B_bc2, writes=[B_bc2])
        wr = sbp("wr", [128, KC, 36], F32); B_wr = Buf("wr")
        P.dma("sp", lambda e: e.dma_start(out=wr[:], in_=w_r.rearrange("(c p) n -> p c n", p=128)), B_wr, writes=[B_wr])
        brb = sbp("brb", [128, 36], F32); B_brb = Buf("brb")
        P.dma("sp", lambda e: e.dma_start(out=brb[:], in_=b_r[0, :].partition_broadcast(128)), B_brb, writes=[B_brb])
        valid = sbp("valid", [128, 1], F32); B_valid = Buf("valid")
        P.op("pool", lambda e: e.memset(valid[:], 1.0), writes=[B_valid])
        P.op("pool", lambda e: e.affine_select(out=valid[:], in_=valid[:], pattern=[[0, 1]], compare_op=ALU.is_ge,
                                               fill=0.0, base=SD - 1, channel_multiplier=-1), reads=[B_valid], writes=[B_valid])
        x2t = [sbp(f"x2t{i}", [128, D], F32) for i in range(2)]
        B_x2t = [Buf(f"x2t{i}") for i in range(2)]
        junk = sbp("junk2", [128, D], F32); B_junk = Buf("junk2")
        st = [sbp(f"st2_{i}", [128, 4], F32) for i in range(2)]
        B_st = [Buf(f"st2{i}") for i in range(2)]
        tmp = [sbp(f"tmp2_{i}", [128, D], F32) for i in range(2)]
        B_tmp = [Buf(f"tmp2{i}") for i in range(2)]
        h2b = [sbp(f"h2b{i}", [128, D], BF16) for i in range(2)]
        B_h2b = [Buf(f"h2b{i}") for i in range(2)]
        h2T = [sbp(f"h2T{i}", [128, KC, 128], F32) for i in range(2)]
        B_h2T = [Buf(f"h2T{i}") for i in range(2)]
        ptf = [psp(f"ptf{i}", [128, 4, 128], F32) for i in range(2)]
        B_ptf = [Buf(f"ptf{i}", True) for i in range(2)]
        plg = [psp(f"plg{i}", [128, 36], F32) for i in range(2)]
        B_plg = [Buf(f"plg{i}", True) for i in range(2)]
        rt = [sbp(f"rt{i}", [128, 96], F32) for i in range(2)]
        B_rt = [Buf(f"rt{i}") for i in range(2)]
        npt = 0
        for t in range(NT):
            i = t % 2
            g = 0 if t < 16 else 1
            P.dma("sp", lambda e, t=t, i=i: e.dma_start(out=x2t[i][:], in_=x2_d[128 * t:128 * (t + 1), :]),
                  B_x2t[i], writes=[B_x2t[i]])
            P.op("act", lambda e, i=i: e.activation(out=junk[:], in_=x2t[i][:], func=AF.Square, accum_out=st[i][:, 0:1]),
                 reads=[B_x2t[i]], writes=[B_junk, B_st[i]])
            P.op("dve", lambda e, i=i: e.tensor_scalar(out=st[i][:, 1:2], in0=st[i][:, 0:1], scalar1=1.0 / D, scalar2=EPS,
                                                       op0=ALU.mult, op1=ALU.add), reads=[B_st[i]], writes=[B_st[i]])
            P.op("act", lambda e, i=i: e.activation(out=st[i][:, 2:3], in_=st[i][:, 1:2], func=AF.Sqrt),
                 reads=[B_st[i]], writes=[B_st[i]])
            P.op("dve", lambda e, i=i: e.reciprocal(out=st[i][:, 3:4], in_=st[i][:, 2:3]), reads=[B_st[i]], writes=[B_st[i]])
            P.op("dve", lambda e, i=i, g=g: e.scalar_tensor_tensor(
                out=tmp[i][:], in0=x2t[i][:], scalar=st[i][:, 3:4], in1=bc2[:, g, 0, :], op0=ALU.mult, op1=ALU.mult),
                reads=[B_x2t[i], B_st[i], B_bc2], writes=[B_tmp[i]])
            P.op("pool", lambda e, i=i, g=g: e.tensor_tensor(out=tmp[i][:], in0=tmp[i][:], in1=bc2[:, g, 1, :], op=ALU.add),
                 reads=[B_tmp[i], B_bc2], writes=[B_tmp[i]])
            P.op("act", lambda e, i=i: e.copy(out=h2b[i][:], in_=tmp[i][:]), reads=[B_tmp[i]], writes=[B_h2b[i]])
            P.dma("sp", lambda e, t=t, i=i: e.dma_start(out=h2_d[128 * t:128 * (t + 1), :], in_=h2b[i][:]),
                  B_h2b[i], reads=[B_h2b[i]])
            for q4 in range(4):
                pi = npt % 2; npt += 1
                P.op("pe", [lambda e, i=i, c=c, pi=pi: e.transpose(out=ptf[pi][:, c % 4, :], in_=tmp[i][:, 128 * c:128 * (c + 1)],
                                                                 identity=ident_f[:]) for c in range(4 * q4, 4 * q4 + 4)],
                     reads=[B_tmp[i], B_ident], writes=[B_ptf[pi]])
                P.op("dve", lambda e, i=i, q4=q4, pi=pi: e.tensor_copy(out=h2T[i][:, 4 * q4:4 * q4 + 4, :], in_=ptf[pi][:]),
                     reads=[B_ptf[pi]], writes=[B_h2T[i]])
            P.op("pe", [lambda e, i=i, c=c: e.matmul(out=plg[i][:], lhsT=h2T[i][:, c, :], rhs=wr[:, c, :],
                                                      start=(c == 0), stop=(c == KC - 1)) for c in range(KC)],
                 reads=[B_h2T[i], B_wr], writes=[B_plg[i]])
            r = rt[i]; Br = B_rt[i]
            lg = r[:, 0:36]; gmax = r[:, 36:37]; ngmax = r[:, 37:38]; eg = r[:, 38:42]; gsum = r[:, 42:43]
            pg = r[:, 43:44]; ohg = r[:, 44:48]; pen = r[:, 48:52]; mx8 = r[:, 52:60]; nl1 = r[:, 60:61]
            rr = r[:, 61:62]; dn = r[:, 62:63]; rc = r[:, 63:64]
            el = sbp(f"el_{t}", [128, 4, 8], F32) if t < 2 else None
            if t < 2:
                els = getattr(P, "_els", []); els.append(el); P._els = els
            el = P._els[i]
            B_el = Br
            P.op("dve", lambda e, i=i, lg=lg: e.tensor_tensor(out=lg, in0=plg[i][:], in1=brb[:], op=ALU.add),
                 reads=[B_plg[i], B_brb], writes=[Br])
            P.op("dve", lambda e, lg=lg, gmax=gmax: e.reduce_max(out=gmax, in_=lg[:, 0:4], axis=AX.X), reads=[Br], writes=[Br])
            P.op("dve", lambda e, gmax=gmax, ngmax=ngmax: e.tensor_scalar(out=ngmax, in0=gmax, scalar1=-1.0, scalar2=None,
                                                                         op0=ALU.mult), reads=[Br], writes=[Br])
            P.op("act", lambda e, lg=lg, eg=eg, ngmax=ngmax, gsum=gsum: e.activation(
                out=eg, in_=lg[:, 0:4], func=AF.Exp, bias=ngmax, scale=1.0, accum_out=gsum), reads=[Br], writes=[Br])
            P.op("dve", lambda e, pg=pg, gsum=gsum: e.reciprocal(out=pg, in_=gsum), reads=[Br], writes=[Br])
            P.op("dve", lambda e, ohg=ohg, lg=lg, gmax=gmax: e.tensor_scalar(out=ohg, in0=lg[:, 0:4], scalar1=gmax, scalar2=None,
                                                                            op0=ALU.is_equal), reads=[Br], writes=[Br])
            P.op("dve", lambda e, ohg=ohg, pen=pen: e.tensor_scalar(out=pen, in0=ohg, scalar1=1e30, scalar2=-1e30,
                                                                    op0=ALU.mult, op1=ALU.add), reads=[Br], writes=[Br])
            P.op("dve", lambda e, el=el, lg=lg, pen=pen: e.tensor_tensor(
                out=el[:], in0=lg[:, 4:36].rearrange("p (g x) -> p g x", g=4),
                in1=pen.unsqueeze(2).to_broadcast([128, 4, 8]), op=ALU.add), reads=[Br], writes=[Br])
            elf = el[:].rearrange("p g x -> p (g x)")
            P.op("dve", lambda e, mx8=mx8, elf=elf: e.max(out=mx8, in_=elf), reads=[Br], writes=[Br])
            P.op("dve", lambda e, mx8=mx8, nl1=nl1: e.tensor_scalar(out=nl1, in0=mx8[:, 0:1], scalar1=-1.0, scalar2=None,
                                                                    op0=ALU.mult), reads=[Br], writes=[Br])
            P.op("act", lambda e, mx8=mx8, nl1=nl1, rr=rr: e.activation(out=rr, in_=mx8[:, 1:2], func=AF.Exp, bias=nl1, scale=1.0),
                 reads=[Br], writes=[Br])
            P.op("dve", lambda e, rr=rr, dn=dn: e.tensor_scalar(out=dn, in0=rr, scalar1=1.0, scalar2=None, op0=ALU.add),
                 reads=[Br], writes=[Br])
            P.op("dve", lambda e, dn=dn, rc=rc: e.reciprocal(out=rc, in_=dn), reads=[Br], writes=[Br])
            P.op("dve", lambda e, t=t, rc=rc, pg=pg: e.tensor_tensor(out=wgt_all[:, t, 0:1], in0=rc, in1=pg, op=ALU.mult),
                 reads=[Br, B_wgt], writes=[B_wgt])
            P.op("dve", lambda e, t=t, rr=rr: e.tensor_tensor(out=wgt_all[:, t, 1:2], in0=wgt_all[:, t, 0:1], in1=rr, op=ALU.mult),
                 reads=[Br, B_wgt], writes=[B_wgt])
            for k in range(2):
                P.op("dve", lambda e, t=t, k=k, elf=elf, mx8=mx8: e.tensor_scalar(
                    out=Mk_all[:, t, k, :], in0=elf, scalar1=mx8[:, k:k + 1], scalar2=None, op0=ALU.is_equal),
                    reads=[Br, B_Mk], writes=[B_Mk])
            if t == 16:
                P.op("dve", lambda e, t=t: e.tensor_scalar(out=Mk_all[:, t, :, :], in0=Mk_all[:, t, :, :], scalar1=valid[:, 0:1],
                                                           scalar2=None, op0=ALU.mult), reads=[B_Mk, B_valid], writes=[B_Mk])
        P.end()
    if stage <= 7:
        return nc

    P.begin()
    with ExitStack() as ph:
        def sbp(name, shape, dt):
            return ph.enter_context(nc.sbuf_tensor(name, list(shape), dt))

        def psp(name, shape, dt):
            return ph.enter_context(nc.psum_tensor(name, list(shape), dt))
        U = sbp("U", [128, 128], F32); ones_f = sbp("ones6", [128, 128], F32); B_c6 = Buf("c6")
        P.op("pool", lambda e: e.memset(U[:], 1.0), writes=[B_c6])
        P.op("pool", lambda e: e.affine_select(out=U[:], in_=U[:], pattern=[[1, 128]], compare_op=ALU.is_ge, fill=0.0,
                                               base=-1, channel_multiplier=-1), reads=[B_c6], writes=[B_c6])
        P.op("pool", lambda e: e.memset(ones_f[:], 1.0), reads=[B_c6], writes=[B_c6])
        pidx = sbp("pidx", [128, 4], F32); B_pidx = Buf("pidx")
        P.op("pool", lambda e: e.iota(pidx[:, 0:1], pattern=[[0, 1]], base=0, channel_multiplier=1,
                                      allow_small_or_imprecise_dtypes=True), writes=[B_pidx])
        P.op("dve", lambda e: e.tensor_scalar(out=pidx[:, 1:2], in0=pidx[:, 0:1], scalar1=2.0, scalar2=None, op0=ALU.mult),
             reads=[B_pidx], writes=[B_pidx])
        P.op("dve", lambda e: e.tensor_scalar(out=pidx[:, 2:3], in0=pidx[:, 0:1], scalar1=2.0, scalar2=1.0, op0=ALU.mult,
                                              op1=ALU.add), reads=[B_pidx], writes=[B_pidx])
        P.op("dve", lambda e: e.tensor_scalar(out=pidx[:, 3:4], in0=pidx[:, 0:1], scalar1=float(SD), scalar2=None,
                                              op0=ALU.is_ge), reads=[B_pidx], writes=[B_pidx])
        tmpi = sbp("tmpi", [128, 1], F32)
        P.op("dve", lambda e: e.tensor_scalar(out=tmpi[:], in0=pidx[:, 0:1], scalar1=float(NSLOT), scalar2=None, op0=ALU.add),
             reads=[B_pidx], writes=[B_pidx])
        P.op("dve", lambda e: e.tensor_tensor(out=pidx[:, 3:4], in0=pidx[:, 3:4], in1=tmpi[:], op=ALU.mult),
             reads=[B_pidx], writes=[B_pidx])
        Ms = sbp("Ms", [128, NT, 32], F32); B_Ms = Buf("Ms")
        P.op("dve", lambda e: e.tensor_tensor(out=Ms[:], in0=Mk_all[:, :, 0, :], in1=Mk_all[:, :, 1, :], op=ALU.add),
             reads=[B_Mk], writes=[B_Ms])
        Rall = sbp("Rall", [128, NT, 32], F32); B_R = Buf("Rall")
        pr = [psp(f"pr{i}", [128, 32], F32) for i in range(2)]
        B_pr = [Buf(f"pr{i}", True) for i in range(2)]
        for t in range(NT):
            i = t % 2
            fl = [lambda e, i=i, t2=t2: e.matmul(out=pr[i][:], lhsT=ones_f[:], rhs=Ms[:, t2, :], start=(t2 == 0), stop=False)
                  for t2 in range(t)]
            fl.append(lambda e, i=i, t=t: e.matmul(out=pr[i][:], lhsT=U[:], rhs=Ms[:, t, :], start=(t == 0), stop=True))
            P.op("pe", fl, reads=[B_Ms, B_c6], writes=[B_pr[i]])
            P.op("dve", lambda e, i=i, t=t: e.tensor_copy(out=Rall[:, t, :], in_=pr[i][:]), reads=[B_pr[i]], writes=[B_R])
        pc = psp("pc", [128, 32], F32); B_pc = Buf("pc", True)
        P.op("pe", [lambda e, t2=t2: e.matmul(out=pc[:], lhsT=ones_f[:], rhs=Ms[:, t2, :], start=(t2 == 0), stop=(t2 == NT - 1))
                    for t2 in range(NT)], reads=[B_Ms, B_c6], writes=[B_pc])
        cw = sbp("cw", [128, 8, 32], F32); B_cw = Buf("cw")
        ci = sbp("ci", [128, 2, 32], I32)
        P.op("dve", lambda e: e.tensor_scalar(out=cw[:, 0, :], in0=pc[:], scalar1=127.0, scalar2=None, op0=ALU.add),
             reads=[B_pc], writes=[B_cw])
        P.op("dve", lambda e: e.tensor_copy(out=ci[:, 0, :], in_=cw[:, 0, :]), reads=[B_cw], writes=[B_cw])
        P.op("dve", lambda e: e.tensor_scalar(out=ci[:, 1, :], in0=ci[:, 0, :], scalar1=7, scalar2=7,
                                              op0=ALU.arith_shift_right, op1=ALU.logical_shift_left), reads=[B_cw], writes=[B_cw])
        P.op("dve", lambda e: e.tensor_copy(out=cw[:, 1, :], in_=ci[:, 1, :]), reads=[B_cw], writes=[B_cw])
        P.op("pool", lambda e: e.memset(cw[:, 4, :], 1.0), reads=[B_cw], writes=[B_cw])
        P.op("dve", lambda e: e.tensor_tensor_scan(out=cw[:, 2, :], data0=cw[:, 4, :], data1=cw[:, 1, :], initial=0.0,
                                                   op0=ALU.mult, op1=ALU.add), reads=[B_cw], writes=[B_cw])
        P.op("dve", lambda e: e.tensor_tensor(out=cw[:, 3, :], in0=cw[:, 2, :], in1=cw[:, 1, :], op=ALU.subtract),
             reads=[B_cw], writes=[B_cw])
        basep = sbp("basep", [128, NT, 32], F32); prod = sbp("prod", [128, NT, 2, 32], F32)
        destf = sbp("destf", [128, NT, 2], F32); B_d = Buf("dwork")
        P.op("dve", lambda e: e.tensor_tensor(out=basep[:], in0=Rall[:], in1=cw[:, 3, :].unsqueeze(1).to_broadcast([128, NT, 32]),
                                              op=ALU.add), reads=[B_R, B_cw], writes=[B_d])
        for k in range(2):
            P.op("dve", lambda e, k=k: e.tensor_tensor(out=prod[:, :, k, :], in0=Mk_all[:, :, k, :], in1=basep[:], op=ALU.mult),
                 reads=[B_d, B_Mk], writes=[B_d])
        P.op("dve", lambda e: e.reduce_sum(out=destf[:].rearrange("p t k -> p (t k)"),
                                           in_=prod[:].rearrange("p t k x -> p (t k) x"), axis=AX.X), reads=[B_d], writes=[B_d])
        P.op("dve", lambda e: e.tensor_scalar(out=destf[:, 16, :], in0=destf[:, 16, :], scalar1=pidx[:, 3:4], scalar2=None,
                                              op0=ALU.add), reads=[B_d, B_pidx], writes=[B_d])
        P.op("dve", lambda e: e.tensor_copy(out=desti[:], in_=destf[:]), reads=[B_d], writes=[B_desti])
        thr = sbp("thr", [128, NBLK], F32)
        P.op("pool", lambda e: e.iota(thr[:], pattern=[[128, NBLK]], base=0, channel_multiplier=0,
                                      allow_small_or_imprecise_dtypes=True), writes=[B_d])
        cmp = sbp("cmp", [128, NBLK, 32], F32); blke = sbp("blke", [128, NBLK], F32); wf = sbp("wf", [128, NBLK, 2], F32)
        P.op("dve", lambda e: e.tensor_tensor(out=cmp[:], in0=cw[:, 2, :].unsqueeze(1).to_broadcast([128, NBLK, 32]),
                                              in1=thr[:].unsqueeze(2).to_broadcast([128, NBLK, 32]), op=ALU.is_le),
             reads=[B_cw, B_d], writes=[B_d])
        P.op("dve", lambda e: e.reduce_sum(out=blke[:], in_=cmp[:], axis=AX.X), reads=[B_d], writes=[B_d])
        same = sbp("same", [128, NBLK], F32)
        P.op("pool", lambda e: e.memset(same[:], 0.0), reads=[B_d], writes=[B_d])
        P.op("dve", lambda e: e.tensor_tensor(out=same[:, 1:NBLK], in0=blke[:, 1:NBLK], in1=blke[:, 0:NBLK - 1], op=ALU.is_equal),
             reads=[B_d], writes=[B_d])
        P.op("dve", lambda e: e.scalar_tensor_tensor(out=blke[:], in0=same[:], scalar=40.0, in1=blke[:], op0=ALU.mult, op1=ALU.add),
             reads=[B_d], writes=[B_d])
        P.op("dve", lambda e: e.tensor_scalar(out=blke[:], in0=blke[:], scalar1=256.0, scalar2=None, op0=ALU.mult),
             reads=[B_d], writes=[B_d])
        for hh in range(2):
            P.op("dve", lambda e, hh=hh: e.tensor_scalar(out=wf[:, :, hh], in0=blke[:], scalar1=pidx[:, 1 + hh:2 + hh],
                                                         scalar2=None, op0=ALU.add), reads=[B_d, B_pidx], writes=[B_d])
        P.op("dve", lambda e: e.tensor_copy(out=widx[:], in_=wf[:]), reads=[B_d], writes=[B_widx])
        B_xbd = Buf("xbd")
        zrow = sbp("zrow", [128, D], BF16); B_zrow = Buf("zrow")
        P.op("pool", lambda e: e.memset(zrow[:], 0.0), writes=[B_zrow])
        for bb in range(NBLK + 1):
            P.dma("sp", lambda e, bb=bb: e.dma_start(out=xb_d[128 * bb:128 * (bb + 1), :], in_=zrow[:]), B_zrow,
                  reads=[B_zrow], writes=[B_xbd])
        hrow = [sbp(f"hrow{i}", [128, D], BF16) for i in range(2)]
        B_hrow = [Buf(f"hrow{i}") for i in range(2)]
        for t in range(NT):
            i = t % 2
            P.dma("sp", lambda e, t=t, i=i: e.dma_start(out=hrow[i][:], in_=h2_d[128 * t:128 * (t + 1), :]),
                  B_hrow[i], writes=[B_hrow[i]])
            for k in range(2):
                P.dma("pool", lambda e, t=t, i=i, k=k: e.indirect_dma_start(
                    out=xb_d[:, :], out_offset=bass.IndirectOffsetOnAxis(ap=desti[:, t, k:k + 1], axis=0),
                    in_=hrow[i][:], in_offset=None), B_hrow[i], reads=[B_hrow[i], B_desti], writes=[B_xbd])
        P.end()
    if stage <= 8:
        return nc

    P.begin()
    with ExitStack() as ph:
        def sbp(name, shape, dt):
            return ph.enter_context(nc.sbuf_tensor(name, list(shape), dt))

        def psp(name, shape, dt):
            return ph.enter_context(nc.psum_tensor(name, list(shape), dt))
        NWS = 6
        ws = [sbp(f"ws{i}", [128, 8192], BF16) for i in range(NWS)]
        B_ws = [Buf(f"ws{i}") for i in range(NWS)]
        Xb = [sbp(f"Xb{i}", [128, D], BF16) for i in range(2)]; B_Xb = [Buf(f"Xb{i}") for i in range(2)]
        XT = [sbp(f"XT{i}", [128, KC, 128], BF16) for i in range(2)]; B_XT = [Buf(f"XT{i}") for i in range(2)]
        sg = [sbp(f"sg{i}", [128, 512], F32) for i in range(2)]; B_sg = [Buf(f"sg{i}") for i in range(2)]
        Hb = [sbp(f"Hb{i}", [128, 1024], BF16) for i in range(2)]; B_Hb = [Buf(f"Hb{i}") for i in range(2)]
        HT = [sbp(f"HT{i}", [128, 8, 128], BF16) for i in range(2)]; B_HT = [Buf(f"HT{i}") for i in range(2)]
        ysb = [sbp(f"ysb{i}", [128, D], F32) for i in range(2)]; B_ysb = [Buf(f"ysb{i}") for i in range(2)]
        pG = [psp(f"pG{i}", [128, 512], F32) for i in range(2)]; pU = [psp(f"pU{i}", [128, 512], F32) for i in range(2)]
        pY = [psp(f"pY{i}", [128, 512], F32) for i in range(2)]
        B_pG = [Buf(f"pG{i}", True) for i in range(2)]; B_pU = [Buf(f"pU{i}", True) for i in range(2)]
        B_pY = [Buf(f"pY{i}", True) for i in range(2)]
        ptx = [psp(f"ptx{i}", [128, 4, 128], BF16) for i in range(2)]; B_ptx = [Buf(f"ptx{i}", True) for i in range(2)]
        _bcr = {}

        def bc_reg(e):
            if "r" not in _bcr:
                _bcr["r"] = e.to_reg(8191)
            return _bcr["r"]
        wsrc = {"g": w_eg.rearrange("(r c) n -> r (c n)", c=8), "u": w_eu.rearrange("(r c) n -> r (c n)", c=8),
                "d": w_ed.rearrange("(r c) n -> r (c n)", c=4)}
        nws = 0; npt = 0
        zr7 = sbp("zr7", [128, D], F32); B_zr7 = Buf("zr7")
        P.op("pool", lambda e: e.memset(zr7[:], 0.0), writes=[B_zr7])
        P.dma("sp", lambda e: e.dma_start(out=yb_d[NSLOT:NSLOT + 128, :], in_=zr7[:]), B_zr7, reads=[B_zr7])
        def prep(b):
            i = b % 2
            P.dma("sp", lambda e, b=b, i=i: e.dma_start(out=Xb[i][:], in_=xb_d[128 * b:128 * (b + 1), :]), B_Xb[i], writes=[B_Xb[i]])
            for q4 in range(4):
                pi = cntx["p"] % 2; cntx["p"] += 1
                P.op("pe", [lambda e, i=i, j=j, pi=pi: e.transpose(out=ptx[pi][:, j % 4, :], in_=Xb[i][:, j:D:16],
                                                                 identity=ident_b[:]) for j in range(4 * q4, 4 * q4 + 4)],
                     reads=[B_Xb[i], B_ident], writes=[B_ptx[pi]])
                if q4 % 2 == 0:
                    P.op("act", lambda e, i=i, q4=q4, pi=pi: e.copy(out=XT[i][:, 4 * q4:4 * q4 + 4, :], in_=ptx[pi][:]),
                         reads=[B_ptx[pi]], writes=[B_XT[i]])
                else:
                    P.op("dve", lambda e, i=i, q4=q4, pi=pi: e.tensor_copy(out=XT[i][:, 4 * q4:4 * q4 + 4, :], in_=ptx[pi][:]),
                         reads=[B_ptx[pi]], writes=[B_XT[i]])
        cntx = {"p": 0}
        prep(0)
        for b in range(NBLK):
            i = b % 2
            slots = {}
            for mat in ("g", "u", "d"):
                for hh in range(2):
                    sl = nws % NWS; nws += 1
                    slots[(mat, hh)] = sl
                    P.dma("pool", lambda e, b=b, mat=mat, hh=hh, sl=sl: e.indirect_dma_start(
                        out=ws[sl][:], out_offset=None, in_=wsrc[mat],
                        in_offset=bass.IndirectOffsetOnAxis(ap=widx[:, b, hh:hh + 1], axis=0),
                        bounds_check=bc_reg(e), oob_is_err=False),
                        B_ws[sl], reads=[B_widx], writes=[B_ws[sl]])
            for mat, pp, Bp in (("g", pG, B_pG), ("u", pU, B_pU)):
                for hh in range(2):
                    sl = slots[(mat, hh)]
                    for fh in range(2):
                        P.op("pe", [lambda e, i=i, c=c, hh=hh, fh=fh, sl=sl, pp=pp: e.matmul(
                            out=pp[fh][:], lhsT=XT[i][:, 8 * hh + c, :], rhs=ws[sl][:, 1024 * c + 512 * fh:1024 * c + 512 * fh + 512],
                            start=(hh == 0 and c == 0), stop=(hh == 1 and c == 7)) for c in range(8)],
                            reads=[B_XT[i], B_ws[sl]], writes=[Bp[fh]])
            if b + 1 < NBLK:
                prep(b + 1)
            for fh in range(2):
                P.op("act", lambda e, fh=fh: e.activation(out=sg[fh][:], in_=pG[fh][:], func=AF.Silu),
                     reads=[B_pG[fh]], writes=[B_sg[fh]])
                P.op("dve", lambda e, i=i, fh=fh: e.tensor_tensor(out=Hb[i][:, 512 * fh:512 * (fh + 1)], in0=sg[fh][:],
                                                                  in1=pU[fh][:], op=ALU.mult),
                     reads=[B_sg[fh], B_pU[fh]], writes=[B_Hb[i]])
            for q4 in range(2):
                pi = cntx["p"] % 2; cntx["p"] += 1
                P.op("pe", [lambda e, i=i, j=j, pi=pi: e.transpose(out=ptx[pi][:, j % 4, :], in_=Hb[i][:, j:1024:8],
                                                                 identity=ident_b[:]) for j in range(4 * q4, 4 * q4 + 4)],
                     reads=[B_Hb[i], B_ident], writes=[B_ptx[pi]])
                P.op("act", lambda e, i=i, q4=q4, pi=pi: e.copy(out=HT[i][:, 4 * q4:4 * q4 + 4, :], in_=ptx[pi][:]),
                     reads=[B_ptx[pi]], writes=[B_HT[i]])
            for dh in range(2):
                for q in range(2):
                    fl = []
                    for hh in range(2):
                        sl = slots[("d", hh)]
                        for c in range(4):
                            fl.append(lambda e, i=i, c=c, hh=hh, sl=sl, q=q, dh=dh: e.matmul(
                                out=pY[q][:], lhsT=HT[i][:, 4 * hh + c, :],
                                rhs=ws[sl][:, 2048 * c + 1024 * dh + 512 * q:2048 * c + 1024 * dh + 512 * q + 512],
                                start=(hh == 0 and c == 0), stop=(hh == 1 and c == 3)))
                    P.op("pe", fl, reads=[B_HT[i], B_ws[slots[("d", 0)]], B_ws[slots[("d", 1)]]], writes=[B_pY[q]])
                    col = 1024 * dh + 512 * q
                    if q == 0:
                        P.op("act", lambda e, i=i, q=q, col=col: e.copy(out=ysb[i][:, col:col + 512], in_=pY[q][:]),
                             reads=[B_pY[q]], writes=[B_ysb[i]])
                    else:
                        P.op("dve", lambda e, i=i, q=q, col=col: e.tensor_copy(out=ysb[i][:, col:col + 512], in_=pY[q][:]),
                             reads=[B_pY[q]], writes=[B_ysb[i]])
            P.dma("sp", lambda e, b=b, i=i: e.dma_start(out=yb_d[128 * b:128 * (b + 1), :], in_=ysb[i][:]),
                  B_ysb[i], reads=[B_ysb[i]])
        P.end()
    if stage <= 9:
        return nc

    P.begin()
    with ExitStack() as ph:
        def sbp(name, shape, dt):
            return ph.enter_context(nc.sbuf_tensor(name, list(shape), dt))
        bc3 = sbp("bc3", [128, 3, D], F32); B_bc3 = Buf("bc3")
        for g in range(2):
            P.dma("sp", lambda e, g=g: e.dma_start(out=bc3[:, g, :], in_=modrows[g, 5, :].partition_broadcast(128)),
                  B_bc3, writes=[B_bc3])
        P.dma("sp", lambda e: e.dma_start(out=bc3[:, 2, :], in_=g_fin[0, :].partition_broadcast(128)), B_bc3, writes=[B_bc3])
        y0 = [sbp(f"y0_{i}", [128, D], F32) for i in range(2)]; y1 = [sbp(f"y1_{i}", [128, D], F32) for i in range(2)]
        xx = [sbp(f"xx_{i}", [128, D], F32) for i in range(2)]
        B_y0 = [Buf(f"y0{i}") for i in range(2)]; B_y1 = [Buf(f"y1{i}") for i in range(2)]; B_xx = [Buf(f"xx{i}") for i in range(2)]
        junk = sbp("junk3", [128, D], F32); B_junk = Buf("junk3")
        st = [sbp(f"st3_{i}", [128, 4], F32) for i in range(2)]; B_st = [Buf(f"st3{i}") for i in range(2)]
        for t in range(NT):
            i = t % 2
            g = 0 if t < 16 else 1
            P.dma("pool", lambda e, t=t, i=i: e.indirect_dma_start(
                out=y0[i][:], out_offset=None, in_=yb_d[:, :],
                in_offset=bass.IndirectOffsetOnAxis(ap=desti[:, t, 0:1], axis=0)), B_y0[i], reads=[B_desti], writes=[B_y0[i]])
            P.dma("pool", lambda e, t=t, i=i: e.indirect_dma_start(
                out=y1[i][:], out_offset=None, in_=yb_d[:, :],
                in_offset=bass.IndirectOffsetOnAxis(ap=desti[:, t, 1:2], axis=0)), B_y1[i], reads=[B_desti], writes=[B_y1[i]])
            P.dma("sp", lambda e, t=t, i=i: e.dma_start(out=xx[i][:], in_=x2_d[128 * t:128 * (t + 1), :]), B_xx[i], writes=[B_xx[i]])
            P.op("dve", lambda e, t=t, i=i: e.tensor_scalar(out=y0[i][:], in0=y0[i][:], scalar1=wgt_all[:, t, 0:1], scalar2=None,
                                                            op0=ALU.mult), reads=[B_y0[i], B_wgt], writes=[B_y0[i]])
            P.op("dve", lambda e, t=t, i=i: e.scalar_tensor_tensor(out=y1[i][:], in0=y1[i][:], scalar=wgt_all[:, t, 1:2],
                                                                   in1=y0[i][:], op0=ALU.mult, op1=ALU.add),
                 reads=[B_y0[i], B_y1[i], B_wgt], writes=[B_y1[i]])
            P.op("pool", lambda e, i=i, g=g: e.tensor_tensor(out=y1[i][:], in0=y1[i][:], in1=bc3[:, g, :], op=ALU.mult),
                 reads=[B_y1[i], B_bc3], writes=[B_y1[i]])
            P.op("dve", lambda e, i=i: e.tensor_tensor(out=xx[i][:], in0=xx[i][:], in1=y1[i][:], op=ALU.add),
                 reads=[B_xx[i], B_y1[i]], writes=[B_xx[i]])
            P.op("act", lambda e, i=i: e.activation(out=junk[:], in_=xx[i][:], func=AF.Square, accum_out=st[i][:, 0:1]),
                 reads=[B_xx[i]], writes=[B_junk, B_st[i]])
            P.op("dve", lambda e, i=i: e.tensor_scalar(out=st[i][:, 1:2], in0=st[i][:, 0:1], scalar1=1.0 / D, scalar2=EPS,
                                                       op0=ALU.mult, op1=ALU.add), reads=[B_st[i]], writes=[B_st[i]])
            P.op("act", lambda e, i=i: e.activation(out=st[i][:, 2:3], in_=st[i][:, 1:2], func=AF.Sqrt),
                 reads=[B_st[i]], writes=[B_st[i]])
            P.op("dve", lambda e, i=i: e.reciprocal(out=st[i][:, 3:4], in_=st[i][:, 2:3]), reads=[B_st[i]], writes=[B_st[i]])
            P.op("dve", lambda e, i=i: e.scalar_tensor_tensor(out=y0[i][:], in0=xx[i][:], scalar=st[i][:, 3:4], in1=bc3[:, 2, :],
                                                              op0=ALU.mult, op1=ALU.mult),
                 reads=[B_xx[i], B_st[i], B_bc3, B_y1[i]], writes=[B_y0[i]])
            if t < 16:
                P.dma("sp", lambda e, t=t, i=i: e.dma_start(out=y_p[128 * t:128 * (t + 1), :], in_=y0[i][:]), B_y0[i], reads=[B_y0[i]])
            else:
                P.dma("sp", lambda e, i=i: e.dma_start(out=y_s, in_=y0[i][0:SD, :]), B_y0[i], reads=[B_y0[i]])
        P.end()
    return nc


_NC = None


def kernel(**inp):
    global _NC
    f = lambda a: np.ascontiguousarray(a, dtype=np.float32)
    pos = np.arange(TOK, dtype=np.float32)
    pos[2048:2048 + SD] = 2048 + np.arange(SD)
    half = 64
    inv = np.exp(-math.log(10000.0) * np.arange(half, dtype=np.float32) / half).astype(np.float32)
    ang = pos[:, None] * inv[None, :]
    rope_cs = np.concatenate([np.cos(ang), np.sin(ang)], axis=1).astype(np.float32)
    shared = {
        "w_ada": f(inp["w_ada"][0]), "b_ada": f(inp["b_ada"][0][None]), "g_mix": f(inp["g_mix"][0][None]),
        "w_in": f(inp["w_in"][0]), "w_pa": f(inp["w_pa"][0]), "w_pb": f(inp["w_pb"][0]), "w_out": f(inp["w_out"][0]),
        "lamv": f(np.stack([inp["lam_q1"][0], inp["lam_k1"][0], inp["lam_q2"][0], inp["lam_k2"][0]])),
        "g_subln": f(inp["g_subln"][0].reshape(256, 1)), "g_moe": f(inp["g_moe"][0][None]),
        "w_r": f(np.concatenate([inp["w_rg"][0], inp["w_re"][0]], axis=1)),
        "b_r": f(np.concatenate([inp["b_rg"][0], inp["b_re"][0]])[None]),
        "w_eg": f(inp["w_e_gate"][0].reshape(32 * 2048, 1024)), "w_eu": f(inp["w_e_up"][0].reshape(32 * 2048, 1024)),
        "w_ed": f(inp["w_e_down"][0].reshape(32 * 1024, 2048)), "g_fin": f(inp["g_final"][None]),
        "rope_cs": rope_cs,
    }
    in_maps = []
    for b in range(8):
        m = dict(shared)
        m["x_p"] = f(inp["x_prompt"][b]); m["x_s"] = f(inp["x_sample"][b])
        m["c_sb_k"] = f(inp["cache_sb_k"][0, b].reshape(S, 1024)); m["c_sb_v"] = f(inp["cache_sb_v"][0, b].reshape(S, 1024))
        m["c_df_k"] = f(inp["cache_df_k"][0, b].reshape(S, 1024)); m["c_df_v"] = f(inp["cache_df_v"][0, b].reshape(S, 1024))
        m["c_in"] = f(np.stack([inp["c_prompt"][b], inp["c_sample"][b]]))
        in_maps.append(m)
    if _NC is None:
        _NC = build_program()
    res = run_bass_kernel_spmd(_NC, in_maps, core_ids=list(range(8)))
    R = res.results

    def g(name, shape):
        return np.stack([np.asarray(R[b][name], dtype=np.float32).reshape(shape) for b in range(8)])
    y_p = g("y_p", (S, D)); y_s = g("y_s", (SD, D))
    outs = [y_p, y_s,
            g("o_sbk_p", (S, 8, 128))[None], g("o_sbv_p", (S, 8, 128))[None],
            g("o_dfk_p", (S, 4, 2, 128))[None], g("o_dfv_p", (S, 4, 256))[None],
            g("o_sbk_s", (SD, 8, 128))[None], g("o_sbv_s", (SD, 8, 128))[None],
            g("o_dfk_s", (SD, 4, 2, 128))[None], g("o_dfv_s", (SD, 4, 256))[None]]
    return tuple(outs)
```
